# Optimizing a Trainium2 kernel written in Bass

```python
import math
import jax, jax.numpy as jnp
from jax import lax
import numpy as np

D_MODEL = 1024
BATCH = 4
SEQ = 4096
DEPTH = 1

CHUNK = 64
Q_BLOCK = 128
ROPE_THETA = 10000.0
NORM_EPS = 1e-6
SUBLN_EPS = 1e-5
DA_HEAD_DIM = 64
DA_HEADS = D_MODEL // 256
DA_WIDTH = DA_HEADS * 2 * DA_HEAD_DIM
RWKV_HEAD = 64
RWKV_HEADS = D_MODEL // 128
RWKV_WIDTH = RWKV_HEADS * RWKV_HEAD
DECAY_LORA = 64
AAA_LORA = 64
GATE_LORA = 128
GN_EPS = 64e-5
RWKV_COLS = 3 * RWKV_WIDTH + DECAY_LORA + AAA_LORA + GATE_LORA
IN_COLS = 3 * DA_WIDTH + RWKV_COLS
N_EXPERTS = 256
TOP_K = 8
N_GROUPS = 8
TOPK_GROUPS = 4
EXPERT_HIDDEN = D_MODEL // 4
ROUTED_SCALE = 2.5
MOE_BLOCK = 128

kernel_name = 'hybrid_diffattn_rwkv7_moe_block'


def rms_norm(x, g, eps):
    xf = x.astype(jnp.float32)
    y = xf * lax.rsqrt(jnp.mean(xf * xf, axis=-1, keepdims=True) + eps)
    return (y * g.astype(jnp.float32)).astype(x.dtype)


def rope(x, positions):
    d = x.shape[-1]
    inv_freq = 1.0 / (ROPE_THETA ** (jnp.arange(0, d, 2, dtype=jnp.float32) / d))
    ang = positions.astype(jnp.float32)[:, :, None] * inv_freq
    cos = jnp.cos(ang)[:, :, None, :]
    sin = jnp.sin(ang)[:, :, None, :]
    xf = x.astype(jnp.float32)
    x1, x2 = xf[..., : d // 2], xf[..., d // 2:]
    return jnp.concatenate([x1 * cos - x2 * sin, x2 * cos + x1 * sin], axis=-1).astype(x.dtype)


def token_shift(p, mu):
    prev = jnp.pad(p, ((0, 0), (1, 0), (0, 0)))[:, :-1]
    return p + (prev - p) * mu


def diff_attention(pq, pk, pv, positions, q_norm_g, k_norm_g, lambda_q1, lambda_k1,
                   lambda_q2, lambda_k2, subln_g, lambda_init):
    B, S, _ = pq.shape
    H, d = DA_HEADS, DA_HEAD_DIM
    q = rope(rms_norm(pq.reshape(B, S, 2 * H, d), q_norm_g, NORM_EPS), positions)
    k = rope(rms_norm(pk.reshape(B, S, 2 * H, d), k_norm_g, NORM_EPS), positions)
    q = q.reshape(B, S, H, 2, d).transpose(0, 2, 3, 1, 4)
    k = k.reshape(B, S, H, 2, d).transpose(0, 2, 3, 1, 4)
    v = pv.reshape(B, S, H, 2 * d).transpose(0, 2, 1, 3)
    f32 = jnp.float32
    lam = (jnp.exp(jnp.sum(lambda_q1.astype(f32) * lambda_k1.astype(f32)))
           - jnp.exp(jnp.sum(lambda_q2.astype(f32) * lambda_k2.astype(f32))) + lambda_init)
    scale = d ** -0.5
    outs = []
    for i in range(S // Q_BLOCK):
        q0 = i * Q_BLOCK
        kv_len = q0 + Q_BLOCK
        qb = q[:, :, :, q0:kv_len]
        kb = k[:, :, :, :kv_len]
        vb = v[:, :, :kv_len]
        s = jnp.einsum('bhmqd,bhmkd->bhmqk', qb, kb).astype(f32) * scale
        q_chunk = (q0 + jnp.arange(Q_BLOCK)) // CHUNK
        k_chunk = jnp.arange(kv_len) // CHUNK
        mask = k_chunk[None, :] <= q_chunk[:, None]
        p = jax.nn.softmax(jnp.where(mask, s, -jnp.inf), axis=-1)
        a = p[:, :, 0] - lam * p[:, :, 1]
        outs.append(jnp.einsum('bhqk,bhkv->bhqv', a.astype(vb.dtype), vb))
    o = jnp.concatenate(outs, axis=2)
    o = rms_norm(o, subln_g, SUBLN_EPS) * (1.0 - lambda_init)
    return o.transpose(0, 2, 1, 3).reshape(B, S, H * 2 * d)


def rwkv7_time_mix(p, mu, w_decay0, w_decay2, a0, a2, g2, k_k, k_a, r_k, ln_x_w, ln_x_b):
    B, S, _ = p.shape
    H, N = RWKV_HEADS, RWKV_HEAD
    f32 = jnp.float32
    xs = token_shift(p, mu)
    c1 = RWKV_WIDTH
    xr, xk, xv, xw, xa, xg = jnp.split(
        xs, [c1, 2 * c1, 3 * c1, 3 * c1 + DECAY_LORA, 3 * c1 + DECAY_LORA + AAA_LORA], axis=-1)
    w = -jax.nn.softplus(-(w_decay0 + jnp.tanh(xw) @ w_decay2)) - 0.5
    a = jax.nn.sigmoid(a0 + xa @ a2)
    g = jax.nn.sigmoid(xg) @ g2
    heads = lambda t: t.astype(f32).reshape(B, S, H, N)
    r, k, v, a, w = heads(xr), heads(xk), heads(xv), heads(a), heads(w)
    kk = k * k_k.astype(f32).reshape(H, N)
    kk = kk / jnp.maximum(jnp.linalg.norm(kk, axis=-1, keepdims=True), 1e-12)
    k = k * (1.0 + (a - 1.0) * k_a.astype(f32).reshape(H, N))
    decay = jnp.exp(-jnp.exp(w))

    def step(state, inp):
        r_t, d_t, k_t, v_t, kk_t, a_t = inp
        sa = jnp.einsum('bhvk,bhk->bhv', state, -kk_t)
        state = (state * d_t[:, :, None, :] + sa[..., None] * (kk_t * a_t)[:, :, None, :]
                 + v_t[..., None] * k_t[:, :, None, :])
        return state, jnp.einsum('bhvk,bhk->bhv', state, r_t)

    seq_first = lambda t: jnp.swapaxes(t, 0, 1)
    state0 = jnp.zeros((B, H, N, N), f32)
    _, y = lax.scan(step, state0, (seq_first(r), seq_first(decay), seq_first(k),
                                   seq_first(v), seq_first(kk), seq_first(a)))
    y = seq_first(y)
    mean = jnp.mean(y, axis=-1, keepdims=True)
    var = jnp.mean(jnp.square(y - mean), axis=-1, keepdims=True)
    y = ((y - mean) * lax.rsqrt(var + GN_EPS)).reshape(B, S, RWKV_WIDTH)
    y = y * ln_x_w.astype(f32) + ln_x_b.astype(f32)
    bonus = jnp.sum(r * k * r_k.astype(f32), axis=-1, keepdims=True) * v
    y = (y + bonus.reshape(B, S, RWKV_WIDTH)) * g.astype(f32)
    return y.astype(p.dtype)


def moe_ffn(h, w_router, router_bias, w_expert_up_gate, w_expert_down, w_shared_up_gate, w_shared_down):
    B, S, D = h.shape
    n_tok = B * S
    F = w_expert_down.shape[1]
    f32 = jnp.float32
    hf = h.reshape(n_tok, D)
    scores = jax.nn.sigmoid((hf @ w_router).astype(f32))
    biased = scores + router_bias.astype(f32)
    grp = biased.reshape(n_tok, N_GROUPS, N_EXPERTS // N_GROUPS)
    grp_score = jnp.sum(lax.top_k(grp, 2)[0], axis=-1)
    _, gidx = lax.top_k(grp_score, TOPK_GROUPS)
    gmask = jnp.sum(jax.nn.one_hot(gidx, N_GROUPS, dtype=f32), axis=-2) > 0
    emask = jnp.repeat(gmask, N_EXPERTS // N_GROUPS, axis=-1)
    _, eidx = lax.top_k(jnp.where(emask, biased, -jnp.inf), TOP_K)
    wts = jnp.take_along_axis(scores, eidx, axis=-1)
    wts = wts / jnp.sum(wts, axis=-1, keepdims=True) * ROUTED_SCALE

    nk = n_tok * TOP_K
    flat_e = eidx.reshape(nk)
    flat_tok = (jnp.arange(nk) // TOP_K).astype(jnp.int32)
    flat_w = wts.reshape(nk)
    order = jnp.argsort(flat_e)
    sorted_e = flat_e[order]
    counts = jnp.bincount(flat_e, length=N_EXPERTS)
    offsets = jnp.cumsum(counts) - counts
    pcounts = (counts + MOE_BLOCK - 1) // MOE_BLOCK * MOE_BLOCK
    pends = jnp.cumsum(pcounts)
    pstarts = pends - pcounts
    dest = pstarts[sorted_e] + (jnp.arange(nk) - offsets[sorted_e])
    n_rows = (nk + N_EXPERTS * (MOE_BLOCK - 1) + MOE_BLOCK - 1) // MOE_BLOCK * MOE_BLOCK
    n_blocks = n_rows // MOE_BLOCK
    row_tok = jnp.zeros((n_rows,), jnp.int32).at[dest].set(flat_tok[order])
    row_w = jnp.zeros((n_rows,), flat_w.dtype).at[dest].set(flat_w[order])
    block_start = jnp.arange(n_blocks) * MOE_BLOCK
    block_e = jnp.minimum(jnp.searchsorted(pends, block_start, side='right'), N_EXPERTS - 1)

    def body(acc, blk):
        tok, w, e = blk
        xb = hf[tok]
        gu = xb @ w_expert_up_gate[e]
        hid = jax.nn.silu(gu[:, :F]) * gu[:, F:]
        yb = hid @ w_expert_down[e]
        return acc.at[tok].add((yb * w[:, None]).astype(acc.dtype)), None

    routed, _ = lax.scan(body, jnp.zeros_like(hf),
                         (row_tok.reshape(n_blocks, MOE_BLOCK), row_w.reshape(n_blocks, MOE_BLOCK), block_e))
    gu = hf @ w_shared_up_gate
    shared = (jax.nn.silu(gu[:, :F]) * gu[:, F:]) @ w_shared_down
    return (shared + routed).reshape(B, S, D)


def hybrid_layer(x, c, positions, layer_idx, w_ada, b_ada, norm1_g, w_in, w_gate, b_gate,
                 q_norm_g, k_norm_g, lambda_q1, lambda_k1, lambda_q2, lambda_k2, subln_g,
                 rwkv_mu, w_decay0, w_decay2, a0, a2, g2, k_k, k_a, r_k, ln_x_w, ln_x_b,
                 w_branch_a, w_branch_b, w_out, norm2_g, w_router, router_bias,
                 w_expert_up_gate, w_expert_down, w_shared_up_gate, w_shared_down):
    mod = jax.nn.silu(c) @ w_ada + b_ada
    sh1, sc1, gt1, sh2, sc2, gt2 = [m[:, None, :] for m in jnp.split(mod, 6, axis=-1)]
    h = rms_norm(x, norm1_g, NORM_EPS) * (1.0 + sc1) + sh1
    proj = h @ w_in
    pq, pk, pv, prw = jnp.split(proj, [DA_WIDTH, 2 * DA_WIDTH, 3 * DA_WIDTH], axis=-1)
    lambda_init = 0.8 - 0.6 * math.exp(-0.3 * layer_idx)
    ya = diff_attention(pq, pk, pv, positions, q_norm_g, k_norm_g, lambda_q1, lambda_k1,
                        lambda_q2, lambda_k2, subln_g, lambda_init) @ w_branch_a
    yb = rwkv7_time_mix(prw, rwkv_mu, w_decay0, w_decay2, a0, a2, g2, k_k, k_a, r_k,
                        ln_x_w, ln_x_b) @ w_branch_b
    ga, gb = jnp.split(jax.nn.sigmoid(h @ w_gate + b_gate), 2, axis=-1)
    x = x + gt1 * ((ga * ya + gb * yb) @ w_out)
    h2 = rms_norm(x, norm2_g, NORM_EPS) * (1.0 + sc2) + sh2
    x = x + gt2 * moe_ffn(h2, w_router, router_bias, w_expert_up_gate, w_expert_down,
                          w_shared_up_gate, w_shared_down)
    return x


def setup_inputs(seed: int = 0) -> dict:
    key = jax.random.key(seed)
    ks = iter(jax.random.split(key, 48))
    f32 = jnp.float32
    L, D, F, E = DEPTH, D_MODEL, EXPERT_HIDDEN, N_EXPERTS
    nrm = lambda shape, s: jax.random.normal(next(ks), shape, f32) * s
    gain = lambda shape: 1.0 + jax.random.normal(next(ks), shape, f32) * 0.02
    x = jax.random.normal(next(ks), (BATCH, SEQ, D), f32)
    c = jax.random.normal(next(ks), (BATCH, D), f32)
    offset = jax.random.randint(next(ks), (BATCH, 1), 0, 1024) * CHUNK
    positions = (offset + jnp.arange(SEQ)[None, :]).astype(jnp.int32)
    return {
        'x': x, 'c': c, 'positions': positions,
        'w_ada': nrm((L, D, 6 * D), 0.5 * D ** -0.5),
        'b_ada': nrm((L, 6 * D), 0.02),
        'norm1_g': gain((L, D)),
        'w_in': nrm((L, D, IN_COLS), D ** -0.5),
        'w_gate': nrm((L, D, 2 * D), D ** -0.5),
        'b_gate': nrm((L, 2 * D), 0.02),
        'q_norm_g': gain((L, DA_HEAD_DIM)),
        'k_norm_g': gain((L, DA_HEAD_DIM)),
        'lambda_q1': nrm((L, DA_HEAD_DIM), 0.1),
        'lambda_k1': nrm((L, DA_HEAD_DIM), 0.1),
        'lambda_q2': nrm((L, DA_HEAD_DIM), 0.1),
        'lambda_k2': nrm((L, DA_HEAD_DIM), 0.1),
        'subln_g': gain((L, 2 * DA_HEAD_DIM)),
        'rwkv_mu': jax.random.uniform(next(ks), (L, RWKV_COLS), f32),
        'w_decay0': jax.random.uniform(next(ks), (L, RWKV_WIDTH), f32, -4.0, -0.5),
        'w_decay2': nrm((L, DECAY_LORA, RWKV_WIDTH), 0.5 * DECAY_LORA ** -0.5),
        'a0': nrm((L, RWKV_WIDTH), 0.1),
        'a2': nrm((L, AAA_LORA, RWKV_WIDTH), 0.5 * AAA_LORA ** -0.5),
        'g2': nrm((L, GATE_LORA, RWKV_WIDTH), GATE_LORA ** -0.5),
        'k_k': 0.85 + nrm((L, RWKV_WIDTH), 0.02),
        'k_a': gain((L, RWKV_WIDTH)),
        'r_k': nrm((L, RWKV_HEADS, RWKV_HEAD), 0.1),
        'ln_x_w': gain((L, RWKV_WIDTH)),
        'ln_x_b': nrm((L, RWKV_WIDTH), 0.02),
        'w_branch_a': nrm((L, DA_WIDTH, D), DA_WIDTH ** -0.5),
        'w_branch_b': nrm((L, RWKV_WIDTH, D), RWKV_WIDTH ** -0.5),
        'w_out': nrm((L, D, D), D ** -0.5),
        'norm2_g': gain((L, D)),
        'w_router': nrm((L, D, E), D ** -0.5),
        'router_bias': nrm((L, E), 0.01),
        'w_expert_up_gate': nrm((L, E, D, 2 * F), D ** -0.5),
        'w_expert_down': nrm((L, E, F, D), F ** -0.5),
        'w_shared_up_gate': nrm((L, D, 2 * F), D ** -0.5),
        'w_shared_down': nrm((L, F, D), F ** -0.5),
    }


def reference(x, c, positions, w_ada, b_ada, norm1_g, w_in, w_gate, b_gate,
              q_norm_g, k_norm_g, lambda_q1, lambda_k1, lambda_q2, lambda_k2, subln_g,
              rwkv_mu, w_decay0, w_decay2, a0, a2, g2, k_k, k_a, r_k, ln_x_w, ln_x_b,
              w_branch_a, w_branch_b, w_out, norm2_g, w_router, router_bias,
              w_expert_up_gate, w_expert_down, w_shared_up_gate, w_shared_down):
    for l in range(DEPTH):
        x = hybrid_layer(x, c, positions, l, w_ada[l], b_ada[l], norm1_g[l], w_in[l], w_gate[l], b_gate[l],
                         q_norm_g[l], k_norm_g[l], lambda_q1[l], lambda_k1[l], lambda_q2[l], lambda_k2[l],
                         subln_g[l], rwkv_mu[l], w_decay0[l], w_decay2[l], a0[l], a2[l], g2[l], k_k[l],
                         k_a[l], r_k[l], ln_x_w[l], ln_x_b[l], w_branch_a[l], w_branch_b[l], w_out[l],
                         norm2_g[l], w_router[l], router_bias[l], w_expert_up_gate[l], w_expert_down[l],
                         w_shared_up_gate[l], w_shared_down[l])
    return x
```

```python
import math
import numpy as np
import concourse.bass as bass
import concourse.mybir as mybir
from concourse.bass_utils import run_bass_kernel_spmd

F32 = mybir.dt.float32
BF16 = mybir.dt.bfloat16
I32 = mybir.dt.int32
AF = mybir.ActivationFunctionType
ALU = mybir.AluOpType
AX = mybir.AxisListType

D = 1024
NTOK = 2048
TG = 256
NGP = 8
NGO = 8
NBLK = 2
CAP = 128 * NBLK
NE = 256
DUMMY_Y = NE * CAP


class V:
    __slots__ = ("tile", "ap")

    def __init__(self, tile, ap):
        self.tile = tile
        self.ap = ap

    def __getitem__(self, idx):
        return V(self.tile, self.ap[idx])

    def re(self, pat, **kw):
        return V(self.tile, self.ap.rearrange(pat, **kw))

    def bc(self, shape):
        return V(self.tile, self.ap.to_broadcast(list(shape)))

    def cast(self, dt):
        return V(self.tile, self.ap.bitcast(dt))


class Tile:
    def __init__(self, name, full, space):
        self.name = name
        self.full = full
        self.space = space
        self.wev = {}
        self.rev = {}

    def __getitem__(self, idx):
        return V(self, self.full[idx])

    @property
    def v(self):
        return V(self, self.full)


class SubTile(Tile):
    def __init__(self, parent, name, full):
        self.parent = parent
        self.name = name
        self.full = full
        self.space = parent.space

    wev = property(lambda s: s.parent.wev, lambda s, v: setattr(s.parent, "wev", v))
    rev = property(lambda s: s.parent.rev, lambda s, v: setattr(s.parent, "rev", v))


class Op:
    __slots__ = ("idx", "eng", "fn", "deps", "group", "is_dma", "signal", "sem", "val")


class Prog:
    ENGS = ("pe", "dve", "act", "pool", "sp")

    def __init__(self, nc):
        self.nc = nc
        self.ops = []
        self.waitall = set()
        self.latest = {}
        self.arena = None
        self.arena_off = 0
        self.arena_size = 0
        self.uid = 0

    def dram(self, name, shape, dtype, kind="Internal"):
        t = self.nc.dram_tensor(name, list(shape), dtype, kind=kind)
        return Tile(name, t.ap(), "dram")

    def sb(self, name, shape, dtype):
        t = self.nc.alloc_sbuf_tensor(name, list(shape), dtype)
        return Tile(name, t[tuple(slice(None) for _ in shape)], "sbuf")

    def psum(self, name, shape, dtype=F32):
        t = self.nc.alloc_psum_tensor(name, list(shape), dtype)
        return Tile(name, t[tuple(slice(None) for _ in shape)], "psum")

    def sub(self, parent, name, ap):
        return SubTile(parent, name, ap)

    def make_arena(self, nfloats):
        self.arena = self.nc.alloc_sbuf_tensor("arena", [128, nfloats], F32)
        self.arena_size = nfloats
        self.arena_off = 0

    def at(self, name, shape, dtype):
        self.uid += 1
        free = 1
        for s in shape[1:]:
            free *= s
        nf = free if dtype in (F32, I32) else (free + 1) // 2
        assert self.arena_off + nf <= self.arena_size, ("arena overflow", name, self.arena_off, nf)
        ap = self.arena[0:shape[0], self.arena_off:self.arena_off + nf]
        self.arena_off += nf
        if dtype != F32:
            ap = ap.bitcast(dtype)
            ap = ap[:, 0:free]
        if len(shape) == 3:
            ap = ap.rearrange("p (a b) -> p a b", a=shape[1])
        elif len(shape) == 4:
            ap = ap.rearrange("p (a b c) -> p a b c", a=shape[1], b=shape[2])
        t = Tile("%s_%d" % (name, self.uid), ap, "sbuf")
        t.key = name
        return t

    def add(self, eng, fn, reads=(), writes=(), partial=False, dma=None):
        op = Op()
        op.idx = len(self.ops)
        op.eng = eng
        op.fn = fn
        op.is_dma = dma is not None
        op.group = ("dma", dma) if op.is_dma else ("eng", eng)
        op.signal = op.is_dma
        op.sem = None
        op.val = 0
        deps = {}
        for v in reads:
            for g, i in v.tile.wev.items():
                if deps.get(g, -1) < i:
                    deps[g] = i
        for v in writes:
            t = v.tile
            for g, i in t.rev.items():
                if deps.get(g, -1) < i:
                    deps[g] = i
            src_w = t.wev if not partial else getattr(t, "gen0", {})
            for g, i in src_w.items():
                if deps.get(g, -1) < i:
                    deps[g] = i
        if (not op.is_dma) and eng == "pe":
            deps.pop(("eng", "pe"), None)
        op.deps = deps
        for i in deps.values():
            self.ops[i].signal = True
        for v in writes:
            t = v.tile
            if partial:
                t.wev[op.group] = op.idx
            else:
                t.wev = {op.group: op.idx}
                t.gen0 = {op.group: op.idx}
                t.rev = {}
        for v in reads:
            v.tile.rev[op.group] = op.idx
        self.ops.append(op)
        if fn is not None:
            self.latest[op.group] = op.idx
        return op

    def barrier(self):
        snap = dict(self.latest)
        for e in self.ENGS:
            op = self.add(e, None)
            d = dict(snap)
            if e == "pe":
                d.pop(("eng", "pe"), None)
            op.deps = d
            for i in d.values():
                self.ops[i].signal = True

    def emit(self):
        nc = self.nc
        sems = {}
        counts = {}
        for op in self.ops:
            if not op.signal:
                continue
            g = op.group
            if g not in sems:
                sems[g] = nc.alloc_semaphore("s%d" % len(sems))
                counts[g] = 0
            counts[g] += 16 if op.is_dma else 1
            op.sem = sems[g]
            op.val = counts[g]
        self.nsems = len(sems)
        per_eng = {e: [] for e in self.ENGS}
        for op in self.ops:
            per_eng[op.eng].append(op)
        ops = self.ops
        waitall = self.waitall
        nwaits = [0]

        def run(e, lst):
            known = {}
            for op in lst:
                for g, i in op.deps.items():
                    q = ops[i]
                    if g[0] == "dma" and g == op.group and g[1] in waitall:
                        continue
                    val = counts[g] if (g[0] == "dma" and g[1] in waitall) else q.val
                    if known.get(g, 0) >= val:
                        continue
                    e.wait_ge(sems[g], val)
                    nwaits[0] += 1
                    known[g] = val
                if op.fn is None:
                    continue
                ins = op.fn(e)
                if op.signal:
                    ins.then_inc(op.sem, 16 if op.is_dma else 1)

        with nc.Block() as block:
            @block.tensor
            def _(e):
                run(e, per_eng["pe"])

            @block.vector
            def _(e):
                run(e, per_eng["dve"])

            @block.scalar
            def _(e):
                run(e, per_eng["act"])

            @block.gpsimd
            def _(e):
                run(e, per_eng["pool"])

            @block.sync
            def _(e):
                run(e, per_eng["sp"])
        self.nwaits = nwaits[0]

    def dma(self, eng, out, in_, key=None, partial=False):
        st = out.tile if out.tile.space != "dram" else in_.tile
        k = key if key is not None else getattr(st, "key", st.name)
        return self.add(eng, lambda e: e.dma_start(out=out.ap, in_=in_.ap), [in_], [out], partial, dma=k)

    def gather(self, out, src, idx, key=None):
        k = key if key is not None else getattr(out.tile, "key", out.tile.name)
        return self.add("pool", lambda e: e.indirect_dma_start(
            out=out.ap, out_offset=None, in_=src.ap,
            in_offset=bass.IndirectOffsetOnAxis(ap=idx.ap, axis=0)), [src, idx], [out], dma=k)

    def mm(self, out, lhsT, rhs, start=True, stop=True):
        return self.add("pe", lambda e: e.matmul(out.ap, lhsT.ap, rhs.ap, start=start, stop=stop),
                        [lhsT, rhs], [out], partial=(not start))

    def tr(self, out, in_, ident, partial=False):
        return self.add("pe", lambda e: e.transpose(out.ap, in_.ap, ident.ap), [in_, ident], [out], partial)

    def act(self, out, in_, func, bias=None, scale=None, accum=None, partial=False):
        reads = [in_]
        kw = {}
        if bias is not None:
            if isinstance(bias, V):
                reads.append(bias)
                kw["bias"] = bias.ap
            else:
                kw["bias"] = bias
        if scale is not None:
            if isinstance(scale, V):
                reads.append(scale)
                kw["scale"] = scale.ap
            else:
                kw["scale"] = scale
        writes = [out]
        if accum is not None:
            writes.append(accum)
            kw["accum_out"] = accum.ap
        return self.add("act", lambda e: e.activation(out.ap, in_.ap, func, **kw), reads, writes, partial)

    def tt(self, eng, out, in0, in1, op, partial=False):
        return self.add(eng, lambda e: e.tensor_tensor(out.ap, in0.ap, in1.ap, op), [in0, in1], [out], partial)

    def ts(self, eng, out, in0, s1, op0, s2=None, op1=None, partial=False):
        reads = [in0]
        a1 = s1.ap if isinstance(s1, V) else s1
        a2 = s2.ap if isinstance(s2, V) else s2
        if isinstance(s1, V):
            reads.append(s1)
        if isinstance(s2, V):
            reads.append(s2)
        kw = {}
        if op1 is not None:
            kw["op1"] = op1
        return self.add(eng, lambda e: e.tensor_scalar(out.ap, in0.ap, a1, a2, op0, **kw), reads, [out], partial)

    def stt(self, eng, out, in0, s, in1, op0, op1, partial=False):
        reads = [in0, in1]
        a = s.ap if isinstance(s, V) else s
        if isinstance(s, V):
            reads.append(s)
        return self.add(eng, lambda e: e.scalar_tensor_tensor(out.ap, in0.ap, a, in1.ap, op0, op1),
                        reads, [out], partial)

    def copy(self, eng, out, in_, partial=False):
        if eng == "act":
            return self.add(eng, lambda e: e.copy(out.ap, in_.ap), [in_], [out], partial)
        return self.add(eng, lambda e: e.tensor_copy(out.ap, in_.ap), [in_], [out], partial)

    def memset(self, eng, out, val, partial=False):
        return self.add(eng, lambda e: e.memset(out.ap, val), [], [out], partial)

    def recip(self, out, in_, partial=False):
        return self.add("dve", lambda e: e.reciprocal(out.ap, in_.ap), [in_], [out], partial)

    def reduce(self, out, in_, op=ALU.add, partial=False):
        return self.add("dve", lambda e: e.tensor_reduce(out=out.ap, in_=in_.ap, axis=AX.X, op=op),
                        [in_], [out], partial)

    def max8(self, out, in_, partial=False):
        return self.add("dve", lambda e: e.max(out=out.ap, in_=in_.ap), [in_], [out], partial)

    def wait(self, eng, views):
        return self.add(eng, None, [], views)


def build(stage=99, dbg=False, new=NE):
    nc = bass.Bass("TRN2", target_bir_lowering=False)
    P = Prog(nc)
    dumps = {}

    def din(name, shape, dt=F32):
        return P.dram(name, shape, dt, kind="ExternalInput")

    def dump(name, view, shape, dt=F32):
        if not dbg:
            return
        t = P.dram("dbg_" + name, shape, dt, kind="ExternalOutput")
        dumps[name] = t
        P.dma("sp", t.v, view, key="dbg")

    xo = din("xo", [NTOK, D])
    xp = din("xp", [NTOK, D])
    ccol = din("ccol", [128, 8])
    posd = din("posd", [128, 32], I32)
    validd = din("validd", [128, 1])
    invfd = din("invfd", [1, 32])
    w_ada = din("w_ada", [D, 6 * D])
    bada_row = din("bada_row", [1, 6 * D])
    bada_col = din("bada_col", [128, 48])
    n1g_col = din("n1g_col", [128, 8])
    n2g_col = din("n2g_col", [128, 8])
    w_in = din("w_in", [D, 3328])
    w_gate = din("w_gate", [D, 2048])
    bgate_row = din("bgate_row", [1, 2048])
    qg_row = din("qg_row", [1, 64])
    kg_row = din("kg_row", [1, 64])
    lam_rows = din("lam_rows", [4, 64])
    subg_row = din("subg_row", [1, 128])
    mu_col = din("mu_col", [128, 14])
    w0_col = din("w0_col", [128, 4])
    wd2 = din("wd2", [64, 512])
    a0_col = din("a0_col", [128, 4])
    a2d = din("a2d", [64, 512])
    g2d = din("g2d", [128, 512])
    kk_col = din("kk_col", [128, 4])
    ka_col = din("ka_col", [128, 4])
    rk_col = din("rk_col", [128, 4])
    lnw_col = din("lnw_col", [128, 4])
    lnb_col = din("lnb_col", [128, 4])
    w_ba = din("w_ba", [512, D])
    w_bb = din("w_bb", [512, D])
    w_out = din("w_out", [D, D])
    w_router = din("w_router", [D, NE])
    rbias_row = din("rbias_row", [1, NE])
    w_eug = din("w_eug", [new, D, 512])
    w_ed = din("w_ed", [new, 256, D])
    w_sug = din("w_sug", [D, 512])
    w_sd = din("w_sd", [256, D])
    outd = P.dram("out", [NTOK, D], F32, kind="ExternalOutput")
    h2s = P.dram("h2s", [NTOK + 1, D], BF16)
    wscr = P.dram("wscr", [NTOK + 1, NE], F32)
    bases = P.dram("bases", [NTOK, D], F32)
    yscr = P.dram("yscr", [NE * CAP + 1, D], BF16)
    gts = P.dram("gts", [2, D], F32)

    banks = [P.psum("bank%d" % i, [128, 512]) for i in range(8)]
    r256 = [P.sub(banks[i], "r256_%d" % i, banks[i].full[:, 0:256]) for i in range(4)]
    r128 = [P.sub(banks[i], "r128_%d" % i, banks[i].full[:, 0:128]) for i in range(4)]
    gb = banks[4:8]
    rr = {"b": 0, "a": 0, "c": 0}

    def nb():
        rr["b"] += 1
        return gb[rr["b"] % 4]

    def n256():
        rr["a"] += 1
        return r256[rr["a"] % 4]

    def n128():
        rr["a"] += 1
        return r128[rr["a"] % 4]

    CK = "const"
    P.waitall.add(CK)

    def cload(name, shape, src_view, dt=F32, eng="sp"):
        t = P.sb(name, shape, dt)
        P.dma(eng, t.v, src_view, key=CK)
        return t

    ident = P.sb("ident", [128, 128], F32)
    P.memset("pool", ident.v, 0.0)
    P.add("pool", lambda e: e.affine_select(ident.full, ident.full, [[-1, 128]], ALU.not_equal, 1.0,
                                            base=0, channel_multiplier=1), [ident.v], [ident.v])
    identb = P.sb("identb", [128, 128], BF16)
    P.copy("dve", identb.v, ident.v)
    ones128 = P.sb("ones128", [128, 128], F32)
    P.memset("pool", ones128.v, 1.0)
    MUs = P.sb("MUs", [128, 128], F32)
    MUi = P.sb("MUi", [128, 128], F32)
    MLs = P.sb("MLs", [128, 128], F32)
    P.add("pool", lambda e: e.affine_select(MUs.full, ones128.full, [[1, 128]], ALU.is_gt, 0.0,
                                            base=0, channel_multiplier=-1), [ones128.v], [MUs.v])
    P.add("pool", lambda e: e.affine_select(MUi.full, ones128.full, [[1, 128]], ALU.is_ge, 0.0,
                                            base=0, channel_multiplier=-1), [ones128.v], [MUi.v])
    P.add("pool", lambda e: e.affine_select(MLs.full, ones128.full, [[-1, 128]], ALU.is_gt, 0.0,
                                            base=0, channel_multiplier=1), [ones128.v], [MLs.v])
    BLK1 = P.sb("BLK1", [128, 128], F32)
    P.memset("pool", BLK1.v, 0.0)
    P.memset("pool", BLK1[0:64, 0:64], 1.0)
    P.memset("pool", BLK1[64:128, 64:128], 1.0)
    BLKm = P.sb("BLKm", [128, 128], F32)
    P.ts("dve", BLKm.v, BLK1.v, 1.0 / 64.0, ALU.mult)
    TRISb = P.sb("TRISb", [128, 128], BF16)
    P.copy("dve", TRISb.v, MUs.v)
    ONESb = P.sb("ONESb", [128, 128], BF16)
    P.copy("dve", ONESb.v, ones128.v)

    valid = cload("valid", [128, 1], validd.v)
    epsn = P.sb("epsn", [128, 1], F32)
    P.memset("dve", epsn.v, 1e-6)
    epss = P.sb("epss", [128, 1], F32)
    P.memset("dve", epss.v, 1e-5)
    epsg = P.sb("epsg", [128, 1], F32)
    P.memset("dve", epsg.v, 64e-5)

    def pb(t, sl=None):
        ap = t.full if sl is None else t.full[sl]
        return V(t, ap.partition_broadcast(128))

    qgb = cload("qgb", [128, 64], pb(qg_row))
    kgb = cload("kgb", [128, 64], pb(kg_row))
    invf = cload("invf", [128, 32], pb(invfd))
    sgb = cload("sgb", [128, 128], pb(subg_row))
    P.ts("dve", sgb.v, sgb.v, 1.0 - 0.2, ALU.mult)
    rbb = cload("rbb", [128, NE], pb(rbias_row))
    lamt = P.sb("lamt", [128, 4, 64], F32)
    for i in range(4):
        P.dma("sp", lamt[:, i, :], pb(lam_rows, (slice(i, i + 1), slice(None))), key=CK, partial=(i > 0))
    mu = cload("mu", [128, 14], mu_col.v)
    omu = P.sb("omu", [128, 14], F32)
    P.ts("dve", omu.v, mu.v, -1.0, ALU.mult, 1.0, ALU.add)
    w0c = cload("w0c", [128, 4], w0_col.v)
    a0c = cload("a0c", [128, 4], a0_col.v)
    kkc = cload("kkc", [128, 4], kk_col.v)
    kac = cload("kac", [128, 4], ka_col.v)
    omka = P.sb("omka", [128, 4], F32)
    P.ts("dve", omka.v, kac.v, -1.0, ALU.mult, 1.0, ALU.add)
    rkc = cload("rkc", [128, 4], rk_col.v)
    lnwc = cload("lnwc", [128, 4], lnw_col.v)
    lnbc = cload("lnbc", [128, 4], lnb_col.v)
    wda = P.sb("wda", [128, 512], F32)
    P.dma("sp", wda[0:64, :], wd2.v, key=CK)
    P.dma("sp", wda[64:128, :], a2d.v, key=CK, partial=True)
    g2s = cload("g2s", [128, 512], g2d.v)
    n1g = cload("n1g", [128, 8], n1g_col.v)
    n2g = cload("n2g", [128, 8], n2g_col.v)
    badac = cload("badac", [128, 48], bada_col.v)
    onesrow = P.sb("onesrow", [1, 128], BF16)
    P.memset("dve", onesrow.v, 1.0)
    modc = P.sb("modc", [128, 32], F32)
    kT = P.sb("kT", [128, 4, 2 * NTOK], BF16)
    P.memset("pool", kT.v, 0.0)
    Vp = P.sb("Vp", [128, 32, 4, 129], BF16)
    P.memset("dve", Vp.v, 1.0)
    ARD = P.sb("ARD", [128, 4, 256], F32)
    BtD = P.sb("BtD", [128, 4, 128], F32)
    KtD = P.sb("KtD", [128, 4, 128], F32)
    VtD = P.sb("VtD", [128, 4, 128], F32)
    for t in (ARD, BtD, KtD, VtD):
        P.memset("pool", t.v, 0.0)
    Sst = [[P.sb("S%d_%d" % (j, k), [128, 128], F32) for k in range(2)] for j in range(4)]
    for j in range(4):
        P.memset("pool", Sst[j][0].v, 0.0)
    car_l = P.sb("car_l", [128, 2], F32)
    car_3 = P.sb("car_3", [128, 4, 3], F32)
    P.memset("dve", car_l.v, 0.0)
    P.memset("dve", car_3.v, 0.0)
    lamc = P.sb("lamc", [128, 1], F32)
    selall = P.sb("selall", [128, 16, NE], BF16)

    ARENA_F = 19 * 1024
    P.make_arena(ARENA_F)
    cache = {}

    def A(name, shape, dtype):
        if name not in cache:
            off = P.arena_off
            cache[name] = (P.at(name, shape, dtype), off)
        return cache[name][0]

    def reset(to=0):
        P.barrier()
        P.arena_off = to
        for k in [k for k, (t, off) in cache.items() if off >= to]:
            del cache[k]

    NSLOT = 3
    wring = [P.sb("wring%d" % i, [128, 8, 512], BF16) for i in range(NSLOT)]
    rs = {"i": 0}

    def nslot():
        rs["i"] += 1
        return wring[rs["i"] % NSLOT]

    def wsl(w, c0, n):
        return V(w, w.full.rearrange("(kc p) n -> p kc n", p=128)[:, :, c0:c0 + n])

    def slab(src_view, ncols):
        t = nslot()
        P.dma("pool", t[:, :, 0:ncols], src_view, key=t.name)
        return t

    def slab4(w):
        t = nslot()
        vw = t.v.re("p a b -> p (a b)").re("p (a b) -> p a b", a=4)
        P.dma("pool", vw, V(w, w.full.rearrange("(hc p) n -> p hc n", p=128)), key=t.name)
        return vw

    cc = cload("cc", [128, 8], ccol.v)
    scc = P.sb("scc", [128, 8], F32)
    P.act(scc.v, cc.v, AF.Silu)
    scb = A("scb", [128, 8, 128], F32)
    P.copy("dve", scb.v, scc.v.re("p (k o) -> p k o", o=1).bc([128, 8, 128]))
    zt = A("zt", [128, D], F32)
    P.memset("dve", zt.v, 0.0)
    ztb = A("ztb", [128, D], BF16)
    P.memset("dve", ztb.v, 0.0)
    P.dma("sp", h2s[NTOK:NTOK + 1, :], ztb[0:1, :], key="zr")
    P.dma("sp", wscr[NTOK:NTOK + 1, :], zt[0:1, 0:NE], key="zr")
    P.dma("sp", yscr[DUMMY_Y:DUMMY_Y + 1, :], ztb[0:1, :], key="zr")
    gtb0 = A("gtb0", [128, 2, D], F32)
    P.dma("sp", gtb0[:, 0, :], pb(bada_row, (slice(None), slice(2 * D, 3 * D))), key="gtb0")
    P.dma("sp", gtb0[:, 1, :], pb(bada_row, (slice(None), slice(5 * D, 6 * D))), key="gtb0", partial=True)
    adaring = [A("adar%d" % i, [128, 8, 256], F32) for i in range(2)]
    wadar = w_ada.full.rearrange("(kc p) n -> p kc n", p=128)
    colbank = gb[0]
    ci = 0
    nsl = 0
    for vec in (0, 1, 3, 4):
        for half in range(4):
            c0 = vec * D + half * 256
            t = adaring[nsl % 2]
            nsl += 1
            P.dma("sp", t.v, V(w_ada, wadar[:, :, c0:c0 + 256]))
            for oc in range(2):
                for kc in range(8):
                    P.mm(colbank[:, ci:ci + 1], t[:, kc, oc * 128:(oc + 1) * 128], scc[:, kc:kc + 1],
                         start=(kc == 0), stop=(kc == 7))
                ci += 1
    for k, vec in enumerate((0, 1, 3, 4)):
        P.tt("dve", modc[:, k * 8:(k + 1) * 8], colbank[:, k * 8:(k + 1) * 8], badac[:, vec * 8:(vec + 1) * 8],
             ALU.add, partial=(k > 0))
    P.stt("dve", modc[:, 8:16], modc[:, 8:16], 1.0, n1g.v, ALU.add, ALU.mult, partial=True)
    P.stt("dve", modc[:, 24:32], modc[:, 24:32], 1.0, n2g.v, ALU.add, ALU.mult, partial=True)
    sh1c, G1c, sh2c, G2c = modc[:, 0:8], modc[:, 8:16], modc[:, 16:24], modc[:, 24:32]
    for gi_, vec in enumerate((2, 5)):
        for half in range(4):
            c0 = vec * D + half * 256
            t = adaring[nsl % 2]
            nsl += 1
            P.dma("sp", t.v, V(w_ada, wadar[:, :, c0:c0 + 256]))
            bk = nb()
            for kc in range(8):
                P.mm(bk[:, 0:256], scb[:, kc, :], t[:, kc, :], start=(kc == 0), stop=(kc == 7))
            P.tt("dve", gtb0[:, gi_, half * 256:(half + 1) * 256], bk[:, 0:256],
                 gtb0[:, gi_, half * 256:(half + 1) * 256], ALU.add, partial=True)
    P.dma("sp", gts.v.re("(o a) b -> o a b", o=1), gtb0[0:1, :, :], key="gts")
    lt1 = A("lt1", [128, 2, 64], F32)
    P.tt("dve", lt1[:, 0, :], lamt[:, 0, :], lamt[:, 1, :], ALU.mult)
    P.tt("dve", lt1[:, 1, :], lamt[:, 2, :], lamt[:, 3, :], ALU.mult, partial=True)
    ls = A("ls", [128, 2], F32)
    P.reduce(ls.v, lt1.v)
    le = A("le", [128, 2], F32)
    P.act(le.v, ls.v, AF.Exp)
    P.tt("dve", lamc.v, le[:, 1:2], le[:, 0:1], ALU.subtract)
    P.ts("dve", lamc.v, lamc.v, -0.2, ALU.add)
    dump("modc", modc.v, [128, 32])
    dump("gtb", gtb0.v.re("p a b -> p (a b)"), [128, 2 * D])
    dump("lamc", lamc.v, [128, 1])
    reset()
    if stage <= 0:
        return finish(P, nc, outd, dumps)

    xr3 = {"o": xo.full.rearrange("(t p) d -> t p d", p=128), "p": xp.full.rearrange("(t p) d -> t p d", p=128)}
    xsrc = {"o": xo, "p": xp}

    def rstd_of(xt, junk, nm):
        ssq = A(nm + "ssq", [128, 1], F32)
        P.memset("dve", ssq.v, 0.0)
        P.act(junk.v, xt, AF.Square, accum=ssq.v)
        rstd = A(nm + "rstd", [128, 1], F32)
        P.act(rstd.v, ssq.v, AF.Sqrt, bias=epsn.v, scale=1.0 / D)
        P.recip(rstd.v, rstd.v)
        return rstd

    def qk_norm_rope(src_bank, gain_b, cs, sn, dstT, dcols):
        pk = A("pk", [128, 8, 64], F32)
        P.copy("act", pk.v, src_bank.v.re("p (m d) -> p m d", m=8))
        sq = A("qsq", [128, 8, 64], F32)
        P.tt("dve", sq.v, pk.v, pk.v, ALU.mult)
        s8 = A("s8", [128, 8], F32)
        P.reduce(s8.v, sq.v)
        P.act(s8.v, s8.v, AF.Sqrt, bias=epsn.v, scale=1.0 / 64.0)
        P.recip(s8.v, s8.v)
        P.tt("dve", pk.v, pk.v, s8.v.re("p (m o) -> p m o", o=1).bc([128, 8, 64]), ALU.mult)
        P.tt("dve", pk.v, pk.v, gain_b.v.re("p (o d) -> p o d", o=1).bc([128, 8, 64]), ALU.mult)
        x1 = pk[:, :, 0:32]
        x2 = pk[:, :, 32:64]
        csb = cs.re("p (o d) -> p o d", o=1).bc([128, 8, 32])
        snb = sn.re("p (o d) -> p o d", o=1).bc([128, 8, 32])
        ta = A("ta", [128, 8, 32], F32)
        tb = A("tb", [128, 8, 32], F32)
        kr = A("kr", [128, 8, 64], BF16)
        P.tt("dve", ta.v, x1, csb, ALU.mult)
        P.tt("dve", tb.v, x2, snb, ALU.mult)
        P.tt("dve", kr[:, :, 0:32], ta.v, tb.v, ALU.subtract)
        P.tt("dve", ta.v, x2, csb, ALU.mult)
        P.tt("dve", tb.v, x1, snb, ALU.mult)
        P.tt("dve", kr[:, :, 32:64], ta.v, tb.v, ALU.add, partial=True)
        bk = nb()
        bkb = bk.v.cast(BF16)
        krf = kr.v.re("p m d -> p (m d)")
        for pc in range(4):
            P.tr(bkb[:, pc * 128:(pc + 1) * 128], krf[:, pc * 128:(pc + 1) * 128], identb.v, partial=(pc > 0))
        P.copy("act", dstT[:, :, dcols], bkb[:, 0:512].re("p (a t) -> p a t", a=4), partial=True)

    for gi in range(NGP + NGO):
        own = gi >= NGP
        src = "o" if own else "p"
        og = gi - NGP
        lt0 = og * 2 if own else gi * 2
        if gi == NGP:
            for j in range(4):
                P.ts("dve", Sst[j][0].v, Sst[j][0].v, valid.v, ALU.mult)
            P.ts("dve", car_l.v, car_l.v, valid.v, ALU.mult)
            P.ts("dve", car_3.v, car_3.v, valid.v, ALU.mult)
        hT = A("hT", [128, 8, TG], BF16)
        if own:
            qT = A("qT", [128, 4, TG], BF16)
            yrw = A("yrw", [128, 4, TG], BF16)
            oT = A("oT", [128, 4, TG], BF16)
        WM = P.arena_off
        posi = A("posi", [128, 2], I32)
        P.dma("sp", posi.v, posd[:, gi * 2:gi * 2 + 2], key="posi")
        posf_ = A("posf", [128, 2], F32)
        P.copy("dve", posf_.v, posi.v)
        ang = A("ang", [128, 2, 32], F32)
        P.tt("dve", ang.v, posf_.v.re("p (t o) -> p t o", o=1).bc([128, 2, 32]),
             invf.v.re("p (o d) -> p o d", o=1).bc([128, 2, 32]), ALU.mult)
        cst = A("cst", [128, 2, 32], F32)
        snt = A("snt", [128, 2, 32], F32)
        for (dst_, off) in ((snt, 0.0), (cst, 0.25)):
            tq = A("tq", [128, 2, 32], F32)
            P.ts("dve", tq.v, ang.v, 1.0 / (2.0 * math.pi), ALU.mult, off, ALU.add)
            tqi = A("tqi", [128, 2, 32], I32)
            P.copy("dve", tqi.v, tq.v)
            P.copy("dve", tq.v, tqi.v)
            rr_ = A("rr_", [128, 2, 32], F32)
            P.stt("dve", rr_.v, tq.v, -6.28125, ang.v, ALU.mult, ALU.add)
            P.stt("dve", rr_.v, tq.v, -0.0019353071795864769, rr_.v, ALU.mult, ALU.add)
            if off != 0.0:
                P.ts("dve", rr_.v, rr_.v, math.pi / 2.0, ALU.add)
            P.ts("dve", rr_.v, rr_.v, 3.14159, ALU.min, -3.14159, ALU.max)
            P.act(dst_.v, rr_.v, AF.Sin)
        junk = A("junk", [128, D], BF16)
        for t in range(2):
            xt = A("xt%d" % t, [128, D], F32)
            P.dma("sp", xt.v, V(xsrc[src], xr3[src][lt0 + t]))
            rstd = rstd_of(xt.v, junk, "n1")
            xn = A("xn", [128, D], BF16)
            P.act(xn.v, xt.v, AF.Identity, scale=rstd.v)
            for hh in range(2):
                bk = nb()
                bkb = bk.v.cast(BF16)
                for k4 in range(4):
                    kc = hh * 4 + k4
                    P.tr(bkb[:, k4 * 128:(k4 + 1) * 128], xn[:, kc * 128:(kc + 1) * 128], identb.v, partial=(k4 > 0))
                for k4 in range(4):
                    kc = hh * 4 + k4
                    P.act(hT[:, kc, t * 128:(t + 1) * 128], bkb[:, k4 * 128:(k4 + 1) * 128], AF.Identity,
                          bias=sh1c[:, kc:kc + 1], scale=G1c[:, kc:kc + 1], partial=True)
        if dbg and gi == NGP:
            dump("hT", hT.v.re("p a b -> p (a b)"), [128, 8 * TG], BF16)
        wk = slab(wsl(w_in, 512, 512), 512)
        wv = slab(wsl(w_in, 1024, 512), 512)
        for t in range(2):
            T = gi * 2 + t
            tc_ = slice(t * 128, (t + 1) * 128)
            bk = nb()
            for kc in range(8):
                P.mm(bk.v, hT[:, kc, tc_], wk[:, kc, :], start=(kc == 0), stop=(kc == 7))
            qk_norm_rope(bk, kgb, cst[:, t, :], snt[:, t, :], kT, slice(T * 128, (T + 1) * 128))
            bk = nb()
            for kc in range(8):
                P.mm(bk.v, hT[:, kc, tc_], wv[:, kc, :], start=(kc == 0), stop=(kc == 7))
            P.act(Vp[:, T, :, 0:128], bk.v.re("p (h d) -> p h d", h=4), AF.Identity,
                  scale=(1.0 if own else valid.v), partial=True)
            if not own:
                P.ts("dve", Vp[:, T, :, 128:129], Vp[:, T, :, 128:129], valid.v, ALU.mult, partial=True)
        if own:
            wq = slab(wsl(w_in, 0, 512), 512)
            for t in range(2):
                tc_ = slice(t * 128, (t + 1) * 128)
                bk = nb()
                for kc in range(8):
                    P.mm(bk.v, hT[:, kc, tc_], wq[:, kc, :], start=(kc == 0), stop=(kc == 7))
                qk_norm_rope(bk, qgb, cst[:, t, :], snt[:, t, :], qT, tc_)
        if dbg and gi == NGP:
            dump("qT", qT.v.re("p a b -> p (a b)"), [128, 4 * TG], BF16)
            dump("kT", kT.v.re("p a b -> p (a b)"), [128, 4 * 2 * NTOK], BF16)
            dump("Vp", Vp.v.re("p a b c -> p (a b c)"), [128, 32 * 4 * 129], BF16)
        if stage <= 1:
            if gi == NGP:
                return finish(P, nc, outd, dumps)
            reset()
            continue
        reset(WM)

        def shift_evac(bk, dst, mucol, omucol, carry):
            P.act(dst, bk[:, 0:TG], AF.Identity, scale=omucol)
            P.stt("dve", dst[:, 1:TG], bk[:, 0:TG - 1], mucol, dst[:, 1:TG], ALU.mult, ALU.add, partial=True)
            P.stt("dve", dst[:, 0:1], carry, mucol, dst[:, 0:1], ALU.mult, ALU.add, partial=True)
            P.copy("dve", carry, bk[:, TG - 1:TG])

        wl = slab(wsl(w_in, 3072, 256), 256)
        xsl = A("xsl", [128, 2, TG], F32)
        for oc in range(2):
            bk = nb()
            for kc in range(8):
                P.mm(bk[:, 0:TG], wl[:, kc, oc * 128:(oc + 1) * 128], hT[:, kc, :], start=(kc == 0), stop=(kc == 7))
            shift_evac(bk, xsl[:, oc, :], mu[:, 12 + oc:13 + oc], omu[:, 12 + oc:13 + oc], car_l[:, oc:oc + 1])
        tw = A("tw", [64, TG], F32)
        P.act(tw.v, xsl[0:64, 0, :], AF.Tanh)
        sgx = A("sgx", [128, TG], F32)
        P.act(sgx.v, xsl[:, 1, :], AF.Sigmoid)
        for j in range(4):
            w3 = nslot()
            for i3 in range(3):
                P.dma("pool", w3[:, :, i3 * 128:(i3 + 1) * 128], wsl(w_in, 1536 + i3 * 512 + j * 128, 128),
                      key=w3.name, partial=(i3 > 0))
            xs3 = A("xs3", [128, 3, TG], F32)
            for i3 in range(3):
                bk = nb()
                for kc in range(8):
                    P.mm(bk[:, 0:TG], w3[:, kc, i3 * 128:(i3 + 1) * 128], hT[:, kc, :], start=(kc == 0),
                         stop=(kc == 7))
                cidx = i3 * 4 + j
                shift_evac(bk, xs3[:, i3, :], mu[:, cidx:cidx + 1], omu[:, cidx:cidx + 1], car_3[:, j, i3:i3 + 1])
            xr_, xk_, xv_ = xs3[:, 0, :], xs3[:, 1, :], xs3[:, 2, :]
            jc = slice(j * 128, (j + 1) * 128)
            bk = nb()
            P.mm(bk[:, 0:TG], wda[0:64, jc], tw.v)
            ld = A("ld", [128, TG], F32)
            P.act(ld.v, bk[:, 0:TG], AF.Sigmoid, bias=w0c[:, j:j + 1])
            P.ts("dve", ld.v, ld.v, -math.exp(-0.5), ALU.mult)
            bk = nb()
            P.mm(bk[:, 0:TG], wda[64:128, jc], xsl[64:128, 0, :])
            aa = A("aa", [128, TG], F32)
            P.act(aa.v, bk[:, 0:TG], AF.Sigmoid, bias=a0c[:, j:j + 1])
            if own:
                bk = nb()
                P.mm(bk[:, 0:TG], g2s[:, jc], sgx.v)
                gj = A("gj", [128, TG], F32)
                P.copy("act", gj.v, bk[:, 0:TG])
            kk = A("kk", [128, TG], F32)
            P.ts("dve", kk.v, xk_, kkc[:, j:j + 1], ALU.mult)
            sq = A("sq", [128, TG], F32)
            P.tt("dve", sq.v, kk.v, kk.v, ALU.mult)
            bk = nb()
            P.mm(bk[:, 0:TG], BLK1.v, sq.v)
            nrm = A("nrm", [128, TG], F32)
            P.act(nrm.v, bk[:, 0:TG], AF.Sqrt)
            P.ts("dve", nrm.v, nrm.v, 1e-12, ALU.max)
            P.recip(nrm.v, nrm.v)
            P.tt("dve", kk.v, kk.v, nrm.v, ALU.mult)
            km = A("km", [128, TG], F32)
            P.ts("dve", km.v, aa.v, kac[:, j:j + 1], ALU.mult, omka[:, j:j + 1], ALU.add)
            P.tt("dve", km.v, km.v, xk_, ALU.mult)
            bbv = A("bbv", [128, TG], F32)
            P.tt("dve", bbv.v, kk.v, aa.v, ALU.mult)
            ca = A("ca", [128, 4, 64], F32)
            cb = A("cb", [128, 4, 64], F32)
            c3 = lambda v_: v_.re("p (c s) -> p c s", c=4)
            cur = c3(ld.v)
            bufs = [ca, cb]
            for si, sh in enumerate((1, 2, 4, 8, 16, 32)):
                nx = bufs[si % 2]
                P.tt("dve", nx[:, :, sh:64], cur[:, :, sh:64], cur[:, :, 0:64 - sh], ALU.add)
                P.copy("dve", nx[:, :, 0:sh], cur[:, :, 0:sh], partial=True)
                cur = nx.v
            cum = cur
            Dinc = A("Dinc", [128, 4, 64], F32)
            Dinv = A("Dinv", [128, 4, 64], F32)
            Dexc = A("Dexc", [128, 4, 64], F32)
            P.act(Dinc.v, cum, AF.Exp)
            P.act(Dinv.v, cum, AF.Exp, scale=-1.0)
            P.tt("dve", Dexc.v, cum, c3(ld.v), ALU.subtract)
            P.act(Dexc.v, Dexc.v, AF.Exp)
            for hf in range(2):
                rws = slice(hf * 64, hf * 64 + 64)
                cl_ = slice(hf * 64, hf * 64 + 64)
                P.stt("dve", ARD[rws, :, cl_], c3(kk.v)[rws], -1.0, Dexc[rws], ALU.mult, ALU.mult, partial=True)
                if own:
                    P.tt("dve", ARD[rws, :, 128 + hf * 64:128 + hf * 64 + 64], c3(xr_)[rws], Dinc[rws], ALU.mult,
                         partial=True)
                P.tt("dve", BtD[rws, :, cl_], c3(bbv.v)[rws], Dinv[rws], ALU.mult, partial=True)
                P.tt("dve", KtD[rws, :, cl_], c3(km.v)[rws], Dinv[rws], ALU.mult, partial=True)
                P.copy("dve", VtD[rws, :, cl_], c3(xv_)[rws], partial=True)
            if own:
                rkr = A("rkr", [128, TG], F32)
                P.stt("dve", rkr.v, xr_, rkc[:, j:j + 1], km.v, ALU.mult, ALU.mult)
                yfm = A("yfm", [128, TG], F32)
            for c in range(4):
                S0 = Sst[j][(gi * 4 + c) % 2]
                S1 = Sst[j][(gi * 4 + c + 1) % 2]
                par = c % 2
                m128 = lambda nm: A("%s_%d" % (nm, par), [128, 128], F32)
                ps1 = n256()
                P.mm(ps1.v, BtD[:, c, :], ARD[:, c, :])
                X = m128("X0")
                P.tt("dve", X.v, ps1[:, 0:128], MUs.v, ALU.mult)
                ps2 = n256()
                P.mm(ps2.v, KtD[:, c, :], ARD[:, c, :])
                AKt = m128("AKt")
                P.tt("dve", AKt.v, ps2[:, 0:128], MUs.v, ALU.mult)
                if own:
                    RBt = m128("RBt")
                    P.tt("dve", RBt.v, ps1[:, 128:256], MUi.v, ALU.mult)
                    RKt = m128("RKt")
                    P.tt("dve", RKt.v, ps2[:, 128:256], MUi.v, ALU.mult)
                ps3 = n128()
                P.mm(ps3.v, ARD[:, c, 0:128], BtD[:, c, :])
                Y = m128("Y0")
                P.tt("dve", Y.v, ps3.v, MLs.v, ALU.mult)
                Tt = m128("Tt0")
                Tl = m128("Tl0")
                P.tt("dve", Tt.v, X.v, ident.v, ALU.add)
                P.tt("dve", Tl.v, Y.v, ident.v, ALU.add)
                for lvl in range(1, 6):
                    px = n128()
                    P.mm(px.v, Y.v, X.v)
                    Xn = m128("X%d" % lvl)
                    P.copy("act", Xn.v, px.v)
                    if lvl < 5:
                        py = n128()
                        P.mm(py.v, X.v, Y.v)
                        Yn = m128("Y%d" % lvl)
                        P.copy("act", Yn.v, py.v)
                    pt = n128()
                    P.mm(pt.v, Tl.v, Xn.v)
                    Ttn = m128("Tt%d" % lvl)
                    P.tt("dve", Ttn.v, pt.v, Tt.v, ALU.add)
                    if lvl < 5:
                        pl = n128()
                        P.mm(pl.v, Xn.v, Tl.v)
                        Tln = m128("Tl%d" % lvl)
                        P.tt("dve", Tln.v, pl.v, Tl.v, ALU.add)
                        Tl = Tln
                        Y = Yn
                    Tt = Ttn
                    X = Xn
                tms = []
                for nm, srcv in (("Atm", ARD[:, c, 0:128]), ("Btm", BtD[:, c, :]), ("Ktm", KtD[:, c, :]),
                                 ("Vtm", VtD[:, c, :])):
                    pp = n128()
                    P.tr(pp.v, srcv, ident.v)
                    tt_ = m128(nm)
                    P.copy("act", tt_.v, pp.v)
                    tms.append(tt_)
                Atm, Btm, Ktm, Vtm = tms
                pp = n128()
                P.mm(pp.v, Tt.v, Atm.v)
                P1 = m128("P1")
                P.copy("act", P1.v, pp.v)
                pp = n128()
                P.mm(pp.v, AKt.v, Vtm.v)
                W2 = m128("W2")
                P.copy("dve", W2.v, pp.v)
                pp = n128()
                P.mm(pp.v, Tt.v, W2.v)
                P2 = m128("P2")
                P.copy("act", P2.v, pp.v)
                pp = n128()
                P.mm(pp.v, P1.v, Btm.v)
                GT = m128("GT")
                P.tt("dve", GT.v, pp.v, ident.v, ALU.add)
                if own:
                    pp = n128()
                    P.mm(pp.v, P1.v, RBt.v)
                    P3t = m128("P3t")
                    P.tt("dve", P3t.v, pp.v, ARD[:, c, 128:256], ALU.add)
                    py_ = n128()
                    P.mm(py_.v, P2.v, RBt.v, start=True, stop=False)
                    P.mm(py_.v, Vtm.v, RKt.v, start=False, stop=False)
                    P.mm(py_.v, S0.v, P3t.v, start=False, stop=True)
                    P.copy("act", yfm[0:64, c * 64:(c + 1) * 64], py_[0:64, 0:64], partial=True)
                    P.copy("act", yfm[64:128, c * 64:(c + 1) * 64], py_[64:128, 64:128], partial=True)
                pss = n128()
                P.mm(pss.v, Btm.v, P2.v, start=True, stop=False)
                P.mm(pss.v, Ktm.v, Vtm.v, start=False, stop=False)
                P.mm(pss.v, GT.v, S0.v, start=False, stop=True)
                P.act(S1.v, pss.v, AF.Identity, scale=Dinc[:, c, 63:64])
            if own:
                bk = nb()
                P.mm(bk[:, 0:TG], BLKm.v, yfm.v)
                cen = A("cen", [128, TG], F32)
                P.tt("dve", cen.v, yfm.v, bk[:, 0:TG], ALU.subtract)
                sq2 = A("sq2", [128, TG], F32)
                P.tt("dve", sq2.v, cen.v, cen.v, ALU.mult)
                bk = nb()
                P.mm(bk[:, 0:TG], BLKm.v, sq2.v)
                rsg = A("rstdg", [128, TG], F32)
                P.act(rsg.v, bk[:, 0:TG], AF.Sqrt, bias=epsg.v)
                P.recip(rsg.v, rsg.v)
                P.tt("dve", cen.v, cen.v, rsg.v, ALU.mult)
                P.ts("dve", cen.v, cen.v, lnwc[:, j:j + 1], ALU.mult, lnbc[:, j:j + 1], ALU.add)
                bk = nb()
                P.mm(bk[:, 0:TG], BLK1.v, rkr.v)
                bon = A("bon", [128, TG], F32)
                P.tt("dve", bon.v, bk[:, 0:TG], xv_, ALU.mult)
                P.tt("dve", cen.v, cen.v, bon.v, ALU.add)
                P.tt("dve", yrw[:, j, :], cen.v, gj.v, ALU.mult, partial=(j > 0))
        if dbg and gi == NGP:
            dump("yrw", yrw.v.re("p a b -> p (a b)"), [128, 4 * TG], BF16)
        if stage <= 2:
            if gi == NGP:
                return finish(P, nc, outd, dumps)
            reset()
            continue
        if not own:
            reset()
            continue
        reset(WM)

        oatt = A("oatt", [128, 2, 512], BF16)
        osb = A("osb", [128, 4, 129], F32)
        nkb = 16 + og * 2 + 2
        for h in range(4):
            for s in range(2):
                accs = [r256[s * 2 + i] for i in range(2)]
                prs = slice(s * 64, s * 64 + 64)
                for kb in range(nkb):
                    bk = nb()
                    P.mm(bk[:, 0:TG], kT[prs, h, kb * 128:(kb + 1) * 128], qT[prs, h, :])
                    E = A("E%d" % (kb % 2), [128, TG], BF16)
                    P.act(E.v, bk[:, 0:TG], AF.Exp, scale=0.125)
                    for i in range(2):
                        oi = og * 2 + i
                        lastkb = 16 + oi
                        if kb > lastkb:
                            continue
                        if kb == lastkb:
                            P.memset("dve", E[64:128, i * 128:i * 128 + 64], 0.0)
                        P.mm(accs[i][:, 0:129], E[:, i * 128:(i + 1) * 128], Vp[:, kb, h, :],
                             start=(kb == 0), stop=(kb == lastkb))
                for i in range(2):
                    P.copy("act", osb[:, s * 2 + i, :], accs[i][:, 0:129], partial=True)
            for i in range(2):
                r0 = A("r0", [128, 1], F32)
                r1 = A("r1", [128, 1], F32)
                P.recip(r0.v, osb[:, i, 128:129])
                P.recip(r1.v, osb[:, 2 + i, 128:129])
                P.tt("dve", r1.v, r1.v, lamc.v, ALU.mult)
                t0 = A("t0", [128, 128], F32)
                P.ts("dve", t0.v, osb[:, i, 0:128], r0.v, ALU.mult)
                P.stt("dve", t0.v, osb[:, 2 + i, 0:128], r1.v, t0.v, ALU.mult, ALU.add)
                ssq = A("assq", [128, 1], F32)
                P.memset("dve", ssq.v, 0.0)
                junk2 = A("junk2", [128, 128], F32)
                P.act(junk2.v, t0.v, AF.Square, accum=ssq.v)
                P.act(ssq.v, ssq.v, AF.Sqrt, bias=epss.v, scale=1.0 / 128.0)
                P.recip(ssq.v, ssq.v)
                P.stt("dve", oatt[:, i, h * 128:(h + 1) * 128], t0.v, ssq.v, sgb.v, ALU.mult, ALU.mult,
                      partial=True)
        for i in range(2):
            bk = nb()
            bkb = bk.v.cast(BF16)
            for hc in range(4):
                P.tr(bkb[:, hc * 128:(hc + 1) * 128], oatt[:, i, hc * 128:(hc + 1) * 128], identb.v, partial=(hc > 0))
            P.copy("act", oT[:, :, i * 128:(i + 1) * 128], bkb[:, 0:512].re("p (a t) -> p a t", a=4), partial=True)
        if dbg and gi == NGP:
            dump("oT", oT.v.re("p a b -> p (a b)"), [128, 4 * TG], BF16)
        if stage <= 3:
            if gi == NGP:
                return finish(P, nc, outd, dumps)
            reset()
            continue
        reset(WM)

        gtb = A("gtb", [128, 2, D], F32)
        P.dma("sp", gtb[:, 0, :], pb(gts, (slice(0, 1), slice(None))), key="gtb")
        P.dma("sp", gtb[:, 1, :], pb(gts, (slice(1, 2), slice(None))), key="gtb", partial=True)
        x1 = A("x1", [128, 2, D], F32)
        WM3 = P.arena_off
        bgb16 = A("bgb16", [1, 2048], BF16)
        P.dma("pool", bgb16.v, bgate_row.v, key="bgb16")
        gates = A("gates", [128, 2, 2048], BF16)
        for cs in range(4):
            wg = slab(wsl(w_gate, cs * 512, 512), 512)
            for i in range(2):
                bk = nb()
                for kc in range(8):
                    P.mm(bk.v, hT[:, kc, i * 128:(i + 1) * 128], wg[:, kc, :], start=(kc == 0), stop=False)
                P.mm(bk.v, onesrow.v, bgb16[0:1, cs * 512:(cs + 1) * 512], start=False, stop=True)
                P.act(gates[:, i, cs * 512:(cs + 1) * 512], bk.v, AF.Sigmoid, partial=True)
        mm_ = A("mm_", [128, 2, D], F32)
        wba = slab4(w_ba)
        for i in range(2):
            for hf in range(2):
                bk = nb()
                for hc in range(4):
                    P.mm(bk.v, oT[:, hc, i * 128:(i + 1) * 128], wba[:, hc, hf * 512:(hf + 1) * 512],
                         start=(hc == 0), stop=(hc == 3))
                P.tt("dve", mm_[:, i, hf * 512:(hf + 1) * 512], bk.v, gates[:, i, hf * 512:(hf + 1) * 512],
                     ALU.mult, partial=True)
        wbb = slab4(w_bb)
        mb = A("mb", [128, 2, D], BF16)
        for i in range(2):
            for hf in range(2):
                bk = nb()
                for hc in range(4):
                    P.mm(bk.v, yrw[:, hc, i * 128:(i + 1) * 128], wbb[:, hc, hf * 512:(hf + 1) * 512],
                         start=(hc == 0), stop=(hc == 3))
                tmpg = A("tmpg", [128, 512], F32)
                P.tt("dve", tmpg.v, bk.v, gates[:, i, 1024 + hf * 512:1024 + (hf + 1) * 512], ALU.mult)
                P.tt("dve", mb[:, i, hf * 512:(hf + 1) * 512], tmpg.v, mm_[:, i, hf * 512:(hf + 1) * 512],
                     ALU.add, partial=True)
        mT = A("mT", [128, 8, TG], BF16)
        for i in range(2):
            for hh in range(2):
                bk = nb()
                bkb = bk.v.cast(BF16)
                for k4 in range(4):
                    kc = hh * 4 + k4
                    P.tr(bkb[:, k4 * 128:(k4 + 1) * 128], mb[:, i, kc * 128:(kc + 1) * 128], identb.v, partial=(k4 > 0))
                P.copy("act", mT[:, hh * 4:(hh + 1) * 4, i * 128:(i + 1) * 128],
                       bkb[:, 0:512].re("p (a t) -> p a t", a=4), partial=True)
        for i in range(2):
            P.dma("sp", x1[:, i, :], V(xo, xr3["o"][lt0 + i]), key="x1", partial=(i > 0))
        for cs in range(2):
            wo = slab(wsl(w_out, cs * 512, 512), 512)
            for i in range(2):
                bk = nb()
                for kc in range(8):
                    P.mm(bk.v, mT[:, kc, i * 128:(i + 1) * 128], wo[:, kc, :], start=(kc == 0), stop=(kc == 7))
                tmpg = A("tmpg", [128, 512], F32)
                P.tt("dve", tmpg.v, bk.v, gtb[:, 0, cs * 512:(cs + 1) * 512], ALU.mult)
                P.tt("dve", x1[:, i, cs * 512:(cs + 1) * 512], tmpg.v, x1[:, i, cs * 512:(cs + 1) * 512], ALU.add,
                     partial=True)
        if dbg and gi == NGP:
            dump("x1", x1.v.re("p a b -> p (a b)"), [128, 2 * D])
        reset(WM3)
        h2T32 = A("h2T32", [128, 8, TG], F32)
        h2Tb = A("h2Tb", [128, 8, TG], BF16)
        junk = A("junkb", [128, D], BF16)
        for i in range(2):
            rstd = rstd_of(x1[:, i, :], junk, "n2")
            xn2 = A("xn2", [128, D], F32)
            P.act(xn2.v, x1[:, i, :], AF.Identity, scale=rstd.v)
            xnb = A("xnb%d" % i, [128, D], BF16)
            P.copy("dve", xnb.v, xn2.v)
            row0 = (lt0 + i) * 128
            P.dma("sp", h2s[row0:row0 + 128, :], xnb.v, partial=True)
            for hh in range(2):
                bk = nb()
                for k4 in range(4):
                    kc = hh * 4 + k4
                    P.tr(bk[:, k4 * 128:(k4 + 1) * 128], xn2[:, kc * 128:(kc + 1) * 128], ident.v, partial=(k4 > 0))
                for k4 in range(4):
                    kc = hh * 4 + k4
                    P.act(h2T32[:, kc, i * 128:(i + 1) * 128], bk[:, k4 * 128:(k4 + 1) * 128], AF.Identity,
                          bias=sh2c[:, kc:kc + 1], scale=G2c[:, kc:kc + 1], partial=True)
        P.copy("dve", h2Tb.v, h2T32.v)
        wrs = nslot()
        wr32 = wrs.v.re("p a b -> p (a b)").cast(F32).re("p (a b) -> p a b", a=8)
        P.dma("pool", wr32, V(w_router, w_router.full.rearrange("(kc p) n -> p kc n", p=128)), key=wrs.name)
        for i in range(2):
            oi = og * 2 + i
            bk = nb()
            for kc in range(8):
                P.mm(bk[:, 0:NE], h2T32[:, kc, i * 128:(i + 1) * 128], V(wrs, wr32.ap[:, kc, :]), start=(kc == 0),
                     stop=(kc == 7))
            sc = A("sc", [128, NE], F32)
            P.act(sc.v, bk[:, 0:NE], AF.Sigmoid)
            bia = A("bia", [128, 8, 32], F32)
            P.tt("dve", bia.v, sc.v.re("p (g e) -> p g e", g=8), rbb.v.re("p (g e) -> p g e", g=8), ALU.add)
            m8g = A("m8g", [128, 8, 8], F32)
            for g in range(8):
                P.max8(m8g[:, g, :], bia[:, g, :], partial=(g > 0))
            gs = A("gs", [128, 8], F32)
            P.tt("dve", gs.v.re("p (g o) -> p g o", o=1), m8g[:, :, 0:1], m8g[:, :, 1:2], ALU.add)
            gm8 = A("gm8", [128, 8], F32)
            P.max8(gm8.v, gs.v)
            gmask = A("gmask", [128, 8], F32)
            P.ts("dve", gmask.v, gs.v, gm8[:, 3:4], ALU.is_ge)
            neg = A("neg", [128, 8], F32)
            P.ts("dve", neg.v, gmask.v, 1e9, ALU.mult, -1e9, ALU.add)
            msk = A("msk", [128, 8, 32], F32)
            P.tt("dve", msk.v, bia.v, gmask.v.re("p (g o) -> p g o", o=1).bc([128, 8, 32]), ALU.mult)
            P.tt("dve", msk.v, msk.v, neg.v.re("p (g o) -> p g o", o=1).bc([128, 8, 32]), ALU.add)
            mskf = msk.v.re("p g e -> p (g e)")
            e8 = A("e8", [128, 8], F32)
            P.max8(e8.v, mskf)
            sel = A("sel", [128, NE], F32)
            P.ts("dve", sel.v, mskf, e8[:, 7:8], ALU.is_ge)
            P.copy("dve", selall[:, oi, :], sel.v, partial=True)
            wsel = A("wsel", [128, NE], F32)
            P.tt("dve", wsel.v, sel.v, sc.v, ALU.mult)
            den = A("den", [128, 1], F32)
            P.reduce(den.v, wsel.v)
            P.recip(den.v, den.v)
            wdn = A("wdn%d" % i, [128, NE], F32)
            P.ts("dve", wdn.v, wsel.v, den.v, ALU.mult, 2.5, ALU.mult)
            row0 = (lt0 + i) * 128
            P.dma("sp", wscr[row0:row0 + 128, :], wdn.v, partial=True)
        wsu = slab(wsl(w_sug, 0, 512), 512)
        hidT = A("hidT", [128, 2, TG], BF16)
        for i in range(2):
            bk = nb()
            for kc in range(8):
                P.mm(bk.v, h2Tb[:, kc, i * 128:(i + 1) * 128], wsu[:, kc, :], start=(kc == 0), stop=(kc == 7))
            sg = A("sg", [128, 256], F32)
            P.act(sg.v, bk[:, 0:256], AF.Silu)
            hid = A("hid", [128, 256], BF16)
            P.tt("dve", hid.v, sg.v, bk[:, 256:512], ALU.mult)
            bk2 = nb()
            bkb = bk2.v.cast(BF16)
            for fc in range(2):
                P.tr(bkb[:, fc * 128:(fc + 1) * 128], hid[:, fc * 128:(fc + 1) * 128], identb.v, partial=(fc > 0))
            P.copy("act", hidT[:, :, i * 128:(i + 1) * 128], bkb[:, 0:256].re("p (a t) -> p a t", a=2), partial=True)
        wsds = nslot()
        wsd = wsds.v.re("p a b -> p (a b)").re("p (a b) -> p a b", a=4)[:, 0:2, :]
        P.dma("pool", wsd, V(w_sd, w_sd.full.rearrange("(fc p) n -> p fc n", p=128)), key=wsds.name)
        for i in range(2):
            bs = A("bs%d" % i, [128, D], F32)
            for hf in range(2):
                bk = nb()
                for fc in range(2):
                    P.mm(bk.v, hidT[:, fc, i * 128:(i + 1) * 128], wsd[:, fc, hf * 512:(hf + 1) * 512],
                         start=(fc == 0), stop=(fc == 1))
                tmpg = A("tmpg", [128, 512], F32)
                P.tt("dve", tmpg.v, bk.v, gtb[:, 1, hf * 512:(hf + 1) * 512], ALU.mult)
                P.tt("dve", bs[:, hf * 512:(hf + 1) * 512], tmpg.v, x1[:, i, hf * 512:(hf + 1) * 512], ALU.add,
                     partial=(hf > 0))
            row0 = (lt0 + i) * 128
            P.dma("sp", bases[row0:row0 + 128, :], bs.v, partial=True)
        if dbg and gi == NGP:
            dump("h2T32", h2T32.v.re("p a b -> p (a b)"), [128, 8 * TG])
            dump("sel0", selall[:, 0, :], [128, NE], BF16)
        if stage <= 4 and gi == NGP:
            return finish(P, nc, outd, dumps)
        reset()

    if stage <= 4:
        return finish(P, nc, outd, dumps)

    gt2b = A("gt2b", [128, D], F32)
    P.dma("sp", gt2b.v, pb(gts, (slice(1, 2), slice(None))), key="gt2b")
    idxT = A("idxT", [128, NBLK, NE], I32)
    idx8 = A("idx8", [128, 16, 8], I32)
    WM2 = P.arena_off
    revb = A("revb", [128, NTOK], F32)
    P.add("pool", lambda e: e.iota(revb.full, [[-1, NTOK]], base=4096, channel_multiplier=0,
                                   allow_small_or_imprecise_dtypes=True), [], [revb.v])
    eoff = A("eoff", [128, NE], F32)
    P.add("pool", lambda e: e.iota(eoff.full, [[CAP, NE]], base=1, channel_multiplier=0,
                                   allow_small_or_imprecise_dtypes=True), [], [eoff.v])
    keyT = A("keyT", [128, 2, NTOK], F32)
    for eh in range(2):
        for q4 in range(2):
            bk = nb()
            bkb = bk.v.cast(BF16)
            for t8 in range(8):
                ti = q4 * 8 + t8
                P.tr(bkb[:, t8 * 128:(t8 + 1) * 128], selall[:, ti, eh * 128:(eh + 1) * 128], identb.v,
                     partial=(t8 > 0))
            P.tt("dve", keyT[:, eh, q4 * 1024:(q4 + 1) * 1024], bkb[:, 0:1024], revb[:, q4 * 1024:(q4 + 1) * 1024],
                 ALU.mult, partial=True)
    top = A("top", [128, 2, CAP], F32)
    for eh in range(2):
        for r in range(CAP // 8):
            P.max8(top[:, eh, r * 8:(r + 1) * 8], keyT[:, eh, :], partial=True)
            P.add("dve", (lambda eh_, r_: (lambda e: e.match_replace(
                out=keyT.full[:, eh_, :], in_to_replace=top.full[:, eh_, r_ * 8:(r_ + 1) * 8],
                in_values=keyT.full[:, eh_, :], imm_value=0.0)))(eh, r), [top.v, keyT.v], [keyT.v])
    P.ts("dve", top.v, top.v, -1.0, ALU.mult, 4096.0, ALU.add)
    P.ts("dve", top.v, top.v, float(NTOK), ALU.min)
    for eh in range(2):
        for blk in range(NBLK):
            bk = nb()
            P.tr(bk[:, 0:128], top[:, eh, blk * 128:(blk + 1) * 128], ident.v)
            P.copy("dve", idxT[:, blk, eh * 128:(eh + 1) * 128], bk[:, 0:128], partial=(eh + blk > 0))
    for ti in range(16):
        bk = nb()
        P.mm(bk[:, 0:NE], TRISb.v, selall[:, ti, :], start=True, stop=(ti == 0))
        for tp in range(ti):
            P.mm(bk[:, 0:NE], ONESb.v, selall[:, tp, :], start=False, stop=(tp == ti - 1))
        okm = A("okm", [128, NE], F32)
        P.ts("dve", okm.v, bk[:, 0:NE], float(CAP), ALU.is_lt)
        fl = A("fl", [128, NE], F32)
        P.tt("dve", fl.v, bk[:, 0:NE], eoff.v, ALU.add)
        P.tt("dve", fl.v, fl.v, okm.v, ALU.mult)
        P.tt("dve", fl.v, fl.v, selall[:, ti, :], ALU.mult)
        t8v = A("t8v", [128, 8], F32)
        P.max8(t8v.v, fl.v)
        isz = A("isz", [128, 8], F32)
        P.ts("dve", isz.v, t8v.v, 0.0, ALU.is_equal, float(DUMMY_Y + 1), ALU.mult)
        P.stt("dve", t8v.v, t8v.v, -1.0, isz.v, ALU.add, ALU.add)
        P.copy("dve", idx8[:, ti, :], t8v.v, partial=(ti > 0))
    if dbg:
        dump("idxT", idxT.v.re("p a b -> p (a b)"), [128, NBLK * NE], I32)
        dump("idx8", idx8.v.re("p a b -> p (a b)"), [128, 128], I32)
    if stage <= 5:
        return finish(P, nc, outd, dumps)

    wdring = [A("wdr%d" % i, [128, 2, D], BF16) for i in range(3)]
    for e in range(NE):
        wug = slab(V(w_eug, w_eug.full[e].rearrange("(kc p) n -> p kc n", p=128)), 512)
        wd = wdring[e % 3]
        P.dma("pool", wd.v, V(w_ed, w_ed.full[e].rearrange("(fc p) n -> p fc n", p=128)))
        for blk in range(NBLK):
            eb = e * NBLK + blk
            Xe = A("Xe%d" % (eb % 2), [128, D], BF16)
            P.gather(Xe.v, h2s.v, idxT[:, blk, e:e + 1])
            We = A("We%d" % (eb % 2), [128, NE], F32)
            P.gather(We.v, wscr.v, idxT[:, blk, e:e + 1])
            XeT = A("XeT%d" % (eb % 2), [128, 8, 128], BF16)
            for hh in range(2):
                bk = nb()
                bkb = bk.v.cast(BF16)
                for k4 in range(4):
                    kc = hh * 4 + k4
                    P.tr(bkb[:, k4 * 128:(k4 + 1) * 128], Xe[:, kc * 128:(kc + 1) * 128], identb.v, partial=(k4 > 0))
                for k4 in range(4):
                    kc = hh * 4 + k4
                    P.act(XeT[:, kc, :], bkb[:, k4 * 128:(k4 + 1) * 128], AF.Identity,
                          bias=sh2c[:, kc:kc + 1], scale=G2c[:, kc:kc + 1], partial=(kc > 0))
            bk = nb()
            for kc in range(8):
                P.mm(bk.v, XeT[:, kc, :], wug[:, kc, :], start=(kc == 0), stop=(kc == 7))
            sg = A("esg", [128, 256], F32)
            P.act(sg.v, bk[:, 0:256], AF.Silu)
            hid = A("ehid", [128, 256], BF16)
            P.tt("dve", hid.v, sg.v, bk[:, 256:512], ALU.mult)
            bk2 = nb()
            bkb = bk2.v.cast(BF16)
            for fc in range(2):
                P.tr(bkb[:, fc * 128:(fc + 1) * 128], hid[:, fc * 128:(fc + 1) * 128], identb.v, partial=(fc > 0))
            ehT = A("ehT", [128, 2, 128], BF16)
            P.copy("act", ehT.v, bkb[:, 0:256].re("p (a t) -> p a t", a=2))
            ysb = A("ysb%d" % (eb % 2), [128, D], BF16)
            for hf in range(2):
                bk = nb()
                for fc in range(2):
                    P.mm(bk.v, ehT[:, fc, :], wd[:, fc, hf * 512:(hf + 1) * 512], start=(fc == 0), stop=(fc == 1))
                if hf == 0:
                    P.act(ysb[:, 0:512], bk.v, AF.Identity, scale=We[:, e:e + 1])
                else:
                    P.ts("dve", ysb[:, 512:1024], bk.v, We[:, e:e + 1], ALU.mult, partial=True)
            P.dma("sp", yscr[e * CAP + blk * 128:e * CAP + (blk + 1) * 128, :], ysb.v, partial=True)
    reset(WM2)

    for ti in range(16):
        acc = A("acc%d" % (ti % 2), [128, D], F32)
        for j in range(8):
            G = A("G%d" % (j % 4), [128, D], BF16)
            P.gather(G.v, yscr.v, idx8[:, ti, j:j + 1])
            if j == 0:
                P.copy("act", acc.v, G.v)
            else:
                P.tt("dve", acc.v, acc.v, G.v, ALU.add)
        bt = A("bt%d" % (ti % 2), [128, D], F32)
        P.dma("sp", bt.v, bases[ti * 128:(ti + 1) * 128, :])
        P.tt("dve", acc.v, acc.v, gt2b.v, ALU.mult)
        P.tt("dve", bt.v, bt.v, acc.v, ALU.add)
        P.dma("sp", outd[ti * 128:(ti + 1) * 128, :], bt.v, partial=True)
    return finish(P, nc, outd, dumps)


def finish(P, nc, outd, dumps):
    outs = [t.v for t in dumps.values()]
    P.wait("sp", outs + [outd.v])
    P.emit()
    return nc, P, dumps


def col(v, n):
    return np.ascontiguousarray(np.asarray(v, np.float32).reshape(n, 128).T)


def make_in_maps(inp, cores=range(8)):
    f = lambda k: np.ascontiguousarray(np.asarray(inp[k])[0])
    shared = {
        "w_ada": f("w_ada"), "bada_row": f("b_ada").reshape(1, -1), "bada_col": col(f("b_ada"), 48),
        "n1g_col": col(f("norm1_g"), 8), "n2g_col": col(f("norm2_g"), 8),
        "w_in": f("w_in"), "w_gate": f("w_gate"), "bgate_row": f("b_gate").reshape(1, -1),
        "qg_row": f("q_norm_g").reshape(1, -1), "kg_row": f("k_norm_g").reshape(1, -1),
        "lam_rows": np.stack([f("lambda_q1"), f("lambda_k1"), f("lambda_q2"), f("lambda_k2")]).astype(np.float32),
        "subg_row": f("subln_g").reshape(1, -1),
        "mu_col": col(f("rwkv_mu"), 14), "w0_col": col(f("w_decay0"), 4), "wd2": f("w_decay2"),
        "a0_col": col(f("a0"), 4), "a2d": f("a2"), "g2d": f("g2"),
        "kk_col": col(f("k_k"), 4), "ka_col": col(f("k_a"), 4), "rk_col": col(f("r_k").reshape(-1), 4),
        "lnw_col": col(f("ln_x_w"), 4), "lnb_col": col(f("ln_x_b"), 4),
        "w_ba": f("w_branch_a"), "w_bb": f("w_branch_b"), "w_out": f("w_out"),
        "w_router": f("w_router"), "rbias_row": f("router_bias").reshape(1, -1),
        "w_eug": f("w_expert_up_gate"), "w_ed": f("w_expert_down"),
        "w_sug": f("w_shared_up_gate"), "w_sd": f("w_shared_down"),
        "invfd": (1.0 / (10000.0 ** (np.arange(0, 64, 2, dtype=np.float32) / 64.0))).astype(np.float32).reshape(1, 32),
    }
    x = np.asarray(inp["x"])
    c = np.asarray(inp["c"])
    pos = np.asarray(inp["positions"])
    maps = []
    for core in cores:
        b, half = core // 2, core % 2
        m = dict(shared)
        m["xo"] = np.ascontiguousarray(x[b, half * NTOK:(half + 1) * NTOK])
        m["xp"] = np.ascontiguousarray(x[b, 0:NTOK])
        pp = np.concatenate([pos[b, 0:NTOK], pos[b, half * NTOK:(half + 1) * NTOK]]).astype(np.int32)
        m["posd"] = np.ascontiguousarray(pp.reshape(32, 128).T)
        m["ccol"] = col(c[b], 8)
        m["validd"] = np.full((128, 1), float(half), np.float32)
        maps.append(m)
    return maps


_CACHE = {}


def kernel(**inputs):
    if "nc" not in _CACHE:
        _CACHE["nc"] = build()[0]
    nc = _CACHE["nc"]
    maps = make_in_maps(inputs)
    res = run_bass_kernel_spmd(nc, maps, core_ids=list(range(8)))
    out = np.zeros((4, 4096, D), np.float32)
    for core in range(8):
        b, half = core // 2, core % 2
        out[b, half * NTOK:(half + 1) * NTOK] = res.results[core]["out"]
    return out
```

```python
import math
import numpy as np
import concourse.bass as bass
import concourse.mybir as mybir
from concourse.bass_utils import run_bass_kernel_spmd

F32 = mybir.dt.float32
BF16 = mybir.dt.bfloat16
I32 = mybir.dt.int32
AF = mybir.ActivationFunctionType
ALU = mybir.AluOpType
AX = mybir.AxisListType

D = 1024
NTOK = 2048
TG = 256
NGP = 8
NGO = 8
NBLK = 2
CAP = 128 * NBLK
NE = 256
DUMMY_Y = NE * CAP


class V:
    __slots__ = ("tile", "ap")

    def __init__(self, tile, ap):
        self.tile = tile
        self.ap = ap

    def __getitem__(self, idx):
        return V(self.tile, self.ap[idx])

    def re(self, pat, **kw):
        return V(self.tile, self.ap.rearrange(pat, **kw))

    def bc(self, shape):
        return V(self.tile, self.ap.to_broadcast(list(shape)))

    def cast(self, dt):
        return V(self.tile, self.ap.bitcast(dt))


class Tile:
    def __init__(self, name, full, space):
        self.name = name
        self.full = full
        self.space = space
        self.wev = {}
        self.rev = {}

    def __getitem__(self, idx):
        return V(self, self.full[idx])

    @property
    def v(self):
        return V(self, self.full)


class SubTile(Tile):
    def __init__(self, parent, name, full):
        self.parent = parent
        self.name = name
        self.full = full
        self.space = parent.space

    wev = property(lambda s: s.parent.wev, lambda s, v: setattr(s.parent, "wev", v))
    rev = property(lambda s: s.parent.rev, lambda s, v: setattr(s.parent, "rev", v))


class Op:
    __slots__ = ("idx", "eng", "fn", "deps", "group", "is_dma", "signal", "sem", "val")


class Prog:
    ENGS = ("pe", "dve", "act", "pool", "sp")

    def __init__(self, nc):
        self.nc = nc
        self.ops = []
        self.waitall = set()
        self.latest = {}
        self.arena = None
        self.arena_off = 0
        self.arena_size = 0
        self.uid = 0

    def dram(self, name, shape, dtype, kind="Internal"):
        t = self.nc.dram_tensor(name, list(shape), dtype, kind=kind)
        return Tile(name, t.ap(), "dram")

    def sb(self, name, shape, dtype):
        t = self.nc.alloc_sbuf_tensor(name, list(shape), dtype)
        return Tile(name, t[tuple(slice(None) for _ in shape)], "sbuf")

    def psum(self, name, shape, dtype=F32):
        t = self.nc.alloc_psum_tensor(name, list(shape), dtype)
        return Tile(name, t[tuple(slice(None) for _ in shape)], "psum")

    def sub(self, parent, name, ap):
        return SubTile(parent, name, ap)

    def make_arena(self, nfloats):
        self.arena = self.nc.alloc_sbuf_tensor("arena", [128, nfloats], F32)
        self.arena_size = nfloats
        self.arena_off = 0

    def at(self, name, shape, dtype):
        self.uid += 1
        free = 1
        for s in shape[1:]:
            free *= s
        nf = free if dtype in (F32, I32) else (free + 1) // 2
        assert self.arena_off + nf <= self.arena_size, ("arena overflow", name, self.arena_off, nf)
        ap = self.arena[0:shape[0], self.arena_off:self.arena_off + nf]
        self.arena_off += nf
        if dtype != F32:
            ap = ap.bitcast(dtype)
            ap = ap[:, 0:free]
        if len(shape) == 3:
            ap = ap.rearrange("p (a b) -> p a b", a=shape[1])
        elif len(shape) == 4:
            ap = ap.rearrange("p (a b c) -> p a b c", a=shape[1], b=shape[2])
        t = Tile("%s_%d" % (name, self.uid), ap, "sbuf")
        t.key = name
        return t

    def add(self, eng, fn, reads=(), writes=(), partial=False, dma=None):
        op = Op()
        op.idx = len(self.ops)
        op.eng = eng
        op.fn = fn
        op.is_dma = dma is not None
        op.group = ("dma", dma) if op.is_dma else ("eng", eng)
        op.signal = op.is_dma
        op.sem = None
        op.val = 0
        deps = {}
        for v in reads:
            for g, i in v.tile.wev.items():
                if deps.get(g, -1) < i:
                    deps[g] = i
        for v in writes:
            t = v.tile
            for g, i in t.rev.items():
                if deps.get(g, -1) < i:
                    deps[g] = i
            src_w = t.wev if not partial else getattr(t, "gen0", {})
            for g, i in src_w.items():
                if deps.get(g, -1) < i:
                    deps[g] = i
        if (not op.is_dma) and eng == "pe":
            deps.pop(("eng", "pe"), None)
        op.deps = deps
        for i in deps.values():
            self.ops[i].signal = True
        for v in writes:
            t = v.tile
            if partial:
                t.wev[op.group] = op.idx
            else:
                t.wev = {op.group: op.idx}
                t.gen0 = {op.group: op.idx}
                t.rev = {}
        for v in reads:
            v.tile.rev[op.group] = op.idx
        self.ops.append(op)
        if fn is not None:
            self.latest[op.group] = op.idx
        return op

    def barrier(self):
        snap = dict(self.latest)
        for e in self.ENGS:
            op = self.add(e, None)
            d = dict(snap)
            if e == "pe":
                d.pop(("eng", "pe"), None)
            op.deps = d
            for i in d.values():
                self.ops[i].signal = True

    def emit(self):
        nc = self.nc
        sems = {}
        counts = {}
        for op in self.ops:
            if not op.signal:
                continue
            g = op.group
            if g not in sems:
                sems[g] = nc.alloc_semaphore("s%d" % len(sems))
                counts[g] = 0
            counts[g] += 16 if op.is_dma else 1
            op.sem = sems[g]
            op.val = counts[g]
        self.nsems = len(sems)
        per_eng = {e: [] for e in self.ENGS}
        for op in self.ops:
            per_eng[op.eng].append(op)
        ops = self.ops
        waitall = self.waitall
        nwaits = [0]

        def run(e, lst):
            known = {}
            for op in lst:
                for g, i in op.deps.items():
                    q = ops[i]
                    if g[0] == "dma" and g == op.group and g[1] in waitall:
                        continue
                    val = counts[g] if (g[0] == "dma" and g[1] in waitall) else q.val
                    if known.get(g, 0) >= val:
                        continue
                    e.wait_ge(sems[g], val)
                    nwaits[0] += 1
                    known[g] = val
                if op.fn is None:
                    continue
                ins = op.fn(e)
                if op.signal:
                    ins.then_inc(op.sem, 16 if op.is_dma else 1)

        with nc.Block() as block:
            @block.tensor
            def _(e):
                run(e, per_eng["pe"])

            @block.vector
            def _(e):
                run(e, per_eng["dve"])

            @block.scalar
            def _(e):
                run(e, per_eng["act"])

            @block.gpsimd
            def _(e):
                run(e, per_eng["pool"])

            @block.sync
            def _(e):
                run(e, per_eng["sp"])
        self.nwaits = nwaits[0]

    def dma(self, eng, out, in_, key=None, partial=False):
        st = out.tile if out.tile.space != "dram" else in_.tile
        k = key if key is not None else getattr(st, "key", st.name)
        return self.add(eng, lambda e: e.dma_start(out=out.ap, in_=in_.ap), [in_], [out], partial, dma=k)

    def gather(self, out, src, idx, key=None):
        k = key if key is not None else getattr(out.tile, "key", out.tile.name)
        return self.add("pool", lambda e: e.indirect_dma_start(
            out=out.ap, out_offset=None, in_=src.ap,
            in_offset=bass.IndirectOffsetOnAxis(ap=idx.ap, axis=0)), [src, idx], [out], dma=k)

    def mm(self, out, lhsT, rhs, start=True, stop=True):
        return self.add("pe", lambda e: e.matmul(out.ap, lhsT.ap, rhs.ap, start=start, stop=stop),
                        [lhsT, rhs], [out], partial=(not start))

    def tr(self, out, in_, ident, partial=False):
        return self.add("pe", lambda e: e.transpose(out.ap, in_.ap, ident.ap), [in_, ident], [out], partial)

    def act(self, out, in_, func, bias=None, scale=None, accum=None, partial=False):
        reads = [in_]
        kw = {}
        if bias is not None:
            if isinstance(bias, V):
                reads.append(bias)
                kw["bias"] = bias.ap
            else:
                kw["bias"] = bias
        if scale is not None:
            if isinstance(scale, V):
                reads.append(scale)
                kw["scale"] = scale.ap
            else:
                kw["scale"] = scale
        writes = [out]
        if accum is not None:
            writes.append(accum)
            kw["accum_out"] = accum.ap
        return self.add("act", lambda e: e.activation(out.ap, in_.ap, func, **kw), reads, writes, partial)

    def tt(self, eng, out, in0, in1, op, partial=False):
        return self.add(eng, lambda e: e.tensor_tensor(out.ap, in0.ap, in1.ap, op), [in0, in1], [out], partial)

    def ts(self, eng, out, in0, s1, op0, s2=None, op1=None, partial=False):
        reads = [in0]
        a1 = s1.ap if isinstance(s1, V) else s1
        a2 = s2.ap if isinstance(s2, V) else s2
        if isinstance(s1, V):
            reads.append(s1)
        if isinstance(s2, V):
            reads.append(s2)
        kw = {}
        if op1 is not None:
            kw["op1"] = op1
        return self.add(eng, lambda e: e.tensor_scalar(out.ap, in0.ap, a1, a2, op0, **kw), reads, [out], partial)

    def stt(self, eng, out, in0, s, in1, op0, op1, partial=False):
        reads = [in0, in1]
        a = s.ap if isinstance(s, V) else s
        if isinstance(s, V):
            reads.append(s)
        return self.add(eng, lambda e: e.scalar_tensor_tensor(out.ap, in0.ap, a, in1.ap, op0, op1),
                        reads, [out], partial)

    def copy(self, eng, out, in_, partial=False):
        if eng == "act":
            return self.add(eng, lambda e: e.copy(out.ap, in_.ap), [in_], [out], partial)
        return self.add(eng, lambda e: e.tensor_copy(out.ap, in_.ap), [in_], [out], partial)

    def memset(self, eng, out, val, partial=False):
        return self.add(eng, lambda e: e.memset(out.ap, val), [], [out], partial)

    def recip(self, out, in_, partial=False):
        return self.add("dve", lambda e: e.reciprocal(out.ap, in_.ap), [in_], [out], partial)

    def reduce(self, out, in_, op=ALU.add, partial=False):
        return self.add("dve", lambda e: e.tensor_reduce(out=out.ap, in_=in_.ap, axis=AX.X, op=op),
                        [in_], [out], partial)

    def max8(self, out, in_, partial=False):
        return self.add("dve", lambda e: e.max(out=out.ap, in_=in_.ap), [in_], [out], partial)

    def wait(self, eng, views):
        return self.add(eng, None, [], views)


def build(stage=99, dbg=False, new=NE):
    nc = bass.Bass("TRN2", target_bir_lowering=False)
    P = Prog(nc)
    dumps = {}

    def din(name, shape, dt=F32):
        return P.dram(name, shape, dt, kind="ExternalInput")

    def dump(name, view, shape, dt=F32):
        if not dbg:
            return
        t = P.dram("dbg_" + name, shape, dt, kind="ExternalOutput")
        dumps[name] = t
        P.dma("sp", t.v, view, key="dbg")

    xo = din("xo", [NTOK, D])
    xp = din("xp", [NTOK, D])
    ccol = din("ccol", [128, 8])
    posd = din("posd", [128, 32], I32)
    validd = din("validd", [128, 1])
    invfd = din("invfd", [1, 32])
    w_ada = din("w_ada", [D, 6 * D])
    bada_row = din("bada_row", [1, 6 * D])
    bada_col = din("bada_col", [128, 48])
    n1g_col = din("n1g_col", [128, 8])
    n2g_col = din("n2g_col", [128, 8])
    w_in = din("w_in", [D, 3328])
    w_gate = din("w_gate", [D, 2048])
    bgate_row = din("bgate_row", [1, 2048])
    qg_row = din("qg_row", [1, 64])
    kg_row = din("kg_row", [1, 64])
    lam_rows = din("lam_rows", [4, 64])
    subg_row = din("subg_row", [1, 128])
    mu_col = din("mu_col", [128, 14])
    w0_col = din("w0_col", [128, 4])
    wd2 = din("wd2", [64, 512])
    a0_col = din("a0_col", [128, 4])
    a2d = din("a2d", [64, 512])
    g2d = din("g2d", [128, 512])
    kk_col = din("kk_col", [128, 4])
    ka_col = din("ka_col", [128, 4])
    rk_col = din("rk_col", [128, 4])
    lnw_col = din("lnw_col", [128, 4])
    lnb_col = din("lnb_col", [128, 4])
    w_ba = din("w_ba", [512, D])
    w_bb = din("w_bb", [512, D])
    w_out = din("w_out", [D, D])
    w_router = din("w_router", [D, NE])
    rbias_row = din("rbias_row", [1, NE])
    w_eug = din("w_eug", [new, D, 512])
    w_ed = din("w_ed", [new, 256, D])
    w_sug = din("w_sug", [D, 512])
    w_sd = din("w_sd", [256, D])
    outd = P.dram("out", [NTOK, D], F32, kind="ExternalOutput")
    h2s = P.dram("h2s", [NTOK + 1, D], BF16)
    wscr = P.dram("wscr", [NTOK + 1, NE], F32)
    bases = P.dram("bases", [NTOK, D], F32)
    yscr = P.dram("yscr", [NE * CAP + 1, D], BF16)
    gts = P.dram("gts", [2, D], F32)

    banks = [P.psum("bank%d" % i, [128, 512]) for i in range(8)]
    r256 = [P.sub(banks[i], "r256_%d" % i, banks[i].full[:, 0:256]) for i in range(8)]
    r128 = [P.sub(banks[i], "r128_%d" % i, banks[i].full[:, 0:128]) for i in range(8)]
    gb = banks[4:8]
    rr = {"b": 0, "a": 0, "c": 0}

    def nb():
        rr["b"] += 1
        return gb[rr["b"] % 4]

    def n256():
        rr["a"] += 1
        return r256[rr["a"] % 8]

    def n128():
        rr["a"] += 1
        return r128[rr["a"] % 8]

    CK = "const"
    P.waitall.add(CK)

    def cload(name, shape, src_view, dt=F32, eng="sp"):
        t = P.sb(name, shape, dt)
        P.dma(eng, t.v, src_view, key=CK)
        return t

    ident = P.sb("ident", [128, 128], F32)
    P.memset("pool", ident.v, 0.0)
    P.add("pool", lambda e: e.affine_select(ident.full, ident.full, [[-1, 128]], ALU.not_equal, 1.0,
                                            base=0, channel_multiplier=1), [ident.v], [ident.v])
    identb = P.sb("identb", [128, 128], BF16)
    P.copy("dve", identb.v, ident.v)
    ones128 = P.sb("ones128", [128, 128], F32)
    P.memset("pool", ones128.v, 1.0)
    MUs = P.sb("MUs", [128, 128], F32)
    MUi = P.sb("MUi", [128, 128], F32)
    MLs = P.sb("MLs", [128, 128], F32)
    P.add("pool", lambda e: e.affine_select(MUs.full, ones128.full, [[1, 128]], ALU.is_gt, 0.0,
                                            base=0, channel_multiplier=-1), [ones128.v], [MUs.v])
    P.add("pool", lambda e: e.affine_select(MUi.full, ones128.full, [[1, 128]], ALU.is_ge, 0.0,
                                            base=0, channel_multiplier=-1), [ones128.v], [MUi.v])
    P.add("pool", lambda e: e.affine_select(MLs.full, ones128.full, [[-1, 128]], ALU.is_gt, 0.0,
                                            base=0, channel_multiplier=1), [ones128.v], [MLs.v])
    BLK1 = P.sb("BLK1", [128, 128], F32)
    P.memset("pool", BLK1.v, 0.0)
    P.memset("pool", BLK1[0:64, 0:64], 1.0)
    P.memset("pool", BLK1[64:128, 64:128], 1.0)
    BLKm = P.sb("BLKm", [128, 128], F32)
    P.ts("dve", BLKm.v, BLK1.v, 1.0 / 64.0, ALU.mult)
    TRISb = P.sb("TRISb", [128, 128], BF16)
    P.copy("dve", TRISb.v, MUs.v)
    ONESb = P.sb("ONESb", [128, 128], BF16)
    P.copy("dve", ONESb.v, ones128.v)

    valid = cload("valid", [128, 1], validd.v)
    epsn = P.sb("epsn", [128, 1], F32)
    P.memset("dve", epsn.v, 1e-6)
    epss = P.sb("epss", [128, 1], F32)
    P.memset("dve", epss.v, 1e-5)
    epsg = P.sb("epsg", [128, 1], F32)
    P.memset("dve", epsg.v, 64e-5)

    def pb(t, sl=None):
        ap = t.full if sl is None else t.full[sl]
        return V(t, ap.partition_broadcast(128))

    qgb = cload("qgb", [128, 64], pb(qg_row))
    kgb = cload("kgb", [128, 64], pb(kg_row))
    invf = cload("invf", [128, 32], pb(invfd))
    sgb = cload("sgb", [128, 128], pb(subg_row))
    P.ts("dve", sgb.v, sgb.v, 1.0 - 0.2, ALU.mult)
    rbb = cload("rbb", [128, NE], pb(rbias_row))
    lamt = P.sb("lamt", [128, 4, 64], F32)
    for i in range(4):
        P.dma("sp", lamt[:, i, :], pb(lam_rows, (slice(i, i + 1), slice(None))), key=CK, partial=(i > 0))
    mu = cload("mu", [128, 14], mu_col.v)
    omu = P.sb("omu", [128, 14], F32)
    P.ts("dve", omu.v, mu.v, -1.0, ALU.mult, 1.0, ALU.add)
    w0c = cload("w0c", [128, 4], w0_col.v)
    a0c = cload("a0c", [128, 4], a0_col.v)
    kkc = cload("kkc", [128, 4], kk_col.v)
    kac = cload("kac", [128, 4], ka_col.v)
    omka = P.sb("omka", [128, 4], F32)
    P.ts("dve", omka.v, kac.v, -1.0, ALU.mult, 1.0, ALU.add)
    rkc = cload("rkc", [128, 4], rk_col.v)
    lnwc = cload("lnwc", [128, 4], lnw_col.v)
    lnbc = cload("lnbc", [128, 4], lnb_col.v)
    wda = P.sb("wda", [128, 512], F32)
    P.dma("sp", wda[0:64, :], wd2.v, key=CK)
    P.dma("sp", wda[64:128, :], a2d.v, key=CK, partial=True)
    g2s = cload("g2s", [128, 512], g2d.v)
    n1g = cload("n1g", [128, 8], n1g_col.v)
    n2g = cload("n2g", [128, 8], n2g_col.v)
    badac = cload("badac", [128, 48], bada_col.v)
    onesrow = P.sb("onesrow", [1, 128], BF16)
    P.memset("dve", onesrow.v, 1.0)
    modc = P.sb("modc", [128, 32], F32)
    kT = P.sb("kT", [128, 4, 2 * NTOK], BF16)
    P.memset("pool", kT.v, 0.0)
    Vp = P.sb("Vp", [128, 32, 4, 129], BF16)
    P.memset("dve", Vp.v, 1.0)
    ARD = P.sb("ARD", [128, 4, 256], BF16)
    BtD = P.sb("BtD", [128, 4, 128], BF16)
    KtD = P.sb("KtD", [128, 4, 128], BF16)
    VtD = P.sb("VtD", [128, 4, 128], BF16)
    for t in (ARD, BtD, KtD, VtD):
        P.memset("pool", t.v, 0.0)
    Sst = [[P.sb("S%d_%d" % (j, k), [128, 128], F32) for k in range(2)] for j in range(4)]
    for j in range(4):
        P.memset("pool", Sst[j][0].v, 0.0)
    car_l = P.sb("car_l", [128, 2], F32)
    car_3 = P.sb("car_3", [128, 4, 3], F32)
    P.memset("dve", car_l.v, 0.0)
    P.memset("dve", car_3.v, 0.0)
    lamc = P.sb("lamc", [128, 1], F32)
    selall = P.sb("selall", [128, 16, NE], BF16)

    ARENA_F = 19 * 1024
    P.make_arena(ARENA_F)
    cache = {}

    def A(name, shape, dtype):
        if name not in cache:
            off = P.arena_off
            cache[name] = (P.at(name, shape, dtype), off)
        return cache[name][0]

    def reset(to=0):
        P.barrier()
        P.arena_off = to
        for k in [k for k, (t, off) in cache.items() if off >= to]:
            del cache[k]

    NSLOT = 3
    wring = [P.sb("wring%d" % i, [128, 8, 512], BF16) for i in range(NSLOT)]
    rs = {"i": 0}

    def nslot():
        rs["i"] += 1
        return wring[rs["i"] % NSLOT]

    def wsl(w, c0, n):
        return V(w, w.full.rearrange("(kc p) n -> p kc n", p=128)[:, :, c0:c0 + n])

    def slab(src_view, ncols):
        t = nslot()
        P.dma("pool", t[:, :, 0:ncols], src_view, key=t.name)
        return t

    def slab4(w):
        t = nslot()
        vw = t.v.re("p a b -> p (a b)").re("p (a b) -> p a b", a=4)
        P.dma("pool", vw, V(w, w.full.rearrange("(hc p) n -> p hc n", p=128)), key=t.name)
        return vw

    cc = cload("cc", [128, 8], ccol.v)
    scc = P.sb("scc", [128, 8], F32)
    P.act(scc.v, cc.v, AF.Silu)
    scb = A("scb", [128, 8, 128], F32)
    P.copy("dve", scb.v, scc.v.re("p (k o) -> p k o", o=1).bc([128, 8, 128]))
    zt = A("zt", [128, D], F32)
    P.memset("dve", zt.v, 0.0)
    ztb = A("ztb", [128, D], BF16)
    P.memset("dve", ztb.v, 0.0)
    P.dma("sp", h2s[NTOK:NTOK + 1, :], ztb[0:1, :], key="zr")
    P.dma("sp", wscr[NTOK:NTOK + 1, :], zt[0:1, 0:NE], key="zr")
    P.dma("sp", yscr[DUMMY_Y:DUMMY_Y + 1, :], ztb[0:1, :], key="zr")
    gtb0 = A("gtb0", [128, 2, D], F32)
    P.dma("sp", gtb0[:, 0, :], pb(bada_row, (slice(None), slice(2 * D, 3 * D))), key="gtb0")
    P.dma("sp", gtb0[:, 1, :], pb(bada_row, (slice(None), slice(5 * D, 6 * D))), key="gtb0", partial=True)
    adaring = [A("adar%d" % i, [128, 8, 256], F32) for i in range(2)]
    wadar = w_ada.full.rearrange("(kc p) n -> p kc n", p=128)
    colbank = gb[0]
    ci = 0
    nsl = 0
    for vec in (0, 1, 3, 4):
        for half in range(4):
            c0 = vec * D + half * 256
            t = adaring[nsl % 2]
            nsl += 1
            P.dma("sp", t.v, V(w_ada, wadar[:, :, c0:c0 + 256]))
            for oc in range(2):
                for kc in range(8):
                    P.mm(colbank[:, ci:ci + 1], t[:, kc, oc * 128:(oc + 1) * 128], scc[:, kc:kc + 1],
                         start=(kc == 0), stop=(kc == 7))
                ci += 1
    for k, vec in enumerate((0, 1, 3, 4)):
        P.tt("dve", modc[:, k * 8:(k + 1) * 8], colbank[:, k * 8:(k + 1) * 8], badac[:, vec * 8:(vec + 1) * 8],
             ALU.add, partial=(k > 0))
    P.stt("dve", modc[:, 8:16], modc[:, 8:16], 1.0, n1g.v, ALU.add, ALU.mult, partial=True)
    P.stt("dve", modc[:, 24:32], modc[:, 24:32], 1.0, n2g.v, ALU.add, ALU.mult, partial=True)
    sh1c, G1c, sh2c, G2c = modc[:, 0:8], modc[:, 8:16], modc[:, 16:24], modc[:, 24:32]
    for gi_, vec in enumerate((2, 5)):
        for half in range(4):
            c0 = vec * D + half * 256
            t = adaring[nsl % 2]
            nsl += 1
            P.dma("sp", t.v, V(w_ada, wadar[:, :, c0:c0 + 256]))
            bk = nb()
            for kc in range(8):
                P.mm(bk[:, 0:256], scb[:, kc, :], t[:, kc, :], start=(kc == 0), stop=(kc == 7))
            P.tt("dve", gtb0[:, gi_, half * 256:(half + 1) * 256], bk[:, 0:256],
                 gtb0[:, gi_, half * 256:(half + 1) * 256], ALU.add, partial=True)
    P.dma("sp", gts.v.re("(o a) b -> o a b", o=1), gtb0[0:1, :, :], key="gts")
    lt1 = A("lt1", [128, 2, 64], F32)
    P.tt("dve", lt1[:, 0, :], lamt[:, 0, :], lamt[:, 1, :], ALU.mult)
    P.tt("dve", lt1[:, 1, :], lamt[:, 2, :], lamt[:, 3, :], ALU.mult, partial=True)
    ls = A("ls", [128, 2], F32)
    P.reduce(ls.v, lt1.v)
    le = A("le", [128, 2], F32)
    P.act(le.v, ls.v, AF.Exp)
    P.tt("dve", lamc.v, le[:, 1:2], le[:, 0:1], ALU.subtract)
    P.ts("dve", lamc.v, lamc.v, -0.2, ALU.add)
    dump("modc", modc.v, [128, 32])
    dump("gtb", gtb0.v.re("p a b -> p (a b)"), [128, 2 * D])
    dump("lamc", lamc.v, [128, 1])
    reset()
    if stage <= 0:
        return finish(P, nc, outd, dumps)

    xr3 = {"o": xo.full.rearrange("(t p) d -> t p d", p=128), "p": xp.full.rearrange("(t p) d -> t p d", p=128)}
    xsrc = {"o": xo, "p": xp}

    def rstd_of(xt, junk, nm):
        ssq = A(nm + "ssq", [128, 1], F32)
        P.memset("dve", ssq.v, 0.0)
        P.act(junk.v, xt, AF.Square, accum=ssq.v)
        rstd = A(nm + "rstd", [128, 1], F32)
        P.act(rstd.v, ssq.v, AF.Sqrt, bias=epsn.v, scale=1.0 / D)
        P.recip(rstd.v, rstd.v)
        return rstd

    def qk_norm_rope(src_bank, gain_b, cs, sn, dstT, dcols):
        pk = A("pk", [128, 8, 64], F32)
        P.copy("act", pk.v, src_bank.v.re("p (m d) -> p m d", m=8))
        sq = A("qsq", [128, 8, 64], F32)
        P.tt("dve", sq.v, pk.v, pk.v, ALU.mult)
        s8 = A("s8", [128, 8], F32)
        P.reduce(s8.v, sq.v)
        P.act(s8.v, s8.v, AF.Sqrt, bias=epsn.v, scale=1.0 / 64.0)
        P.recip(s8.v, s8.v)
        P.tt("dve", pk.v, pk.v, s8.v.re("p (m o) -> p m o", o=1).bc([128, 8, 64]), ALU.mult)
        P.tt("dve", pk.v, pk.v, gain_b.v.re("p (o d) -> p o d", o=1).bc([128, 8, 64]), ALU.mult)
        x1 = pk[:, :, 0:32]
        x2 = pk[:, :, 32:64]
        csb = cs.re("p (o d) -> p o d", o=1).bc([128, 8, 32])
        snb = sn.re("p (o d) -> p o d", o=1).bc([128, 8, 32])
        ta = A("ta", [128, 8, 32], F32)
        tb = A("tb", [128, 8, 32], F32)
        kr = A("kr", [128, 8, 64], BF16)
        P.tt("dve", ta.v, x1, csb, ALU.mult)
        P.tt("dve", tb.v, x2, snb, ALU.mult)
        P.tt("dve", kr[:, :, 0:32], ta.v, tb.v, ALU.subtract)
        P.tt("dve", ta.v, x2, csb, ALU.mult)
        P.tt("dve", tb.v, x1, snb, ALU.mult)
        P.tt("dve", kr[:, :, 32:64], ta.v, tb.v, ALU.add, partial=True)
        bk = nb()
        bkb = bk.v.cast(BF16)
        krf = kr.v.re("p m d -> p (m d)")
        for pc in range(4):
            P.tr(bkb[:, pc * 128:(pc + 1) * 128], krf[:, pc * 128:(pc + 1) * 128], identb.v, partial=(pc > 0))
        P.copy("act", dstT[:, :, dcols], bkb[:, 0:512].re("p (a t) -> p a t", a=4), partial=True)

    for gi in range(NGP + NGO):
        own = gi >= NGP
        src = "o" if own else "p"
        og = gi - NGP
        lt0 = og * 2 if own else gi * 2
        if gi == NGP:
            for j in range(4):
                P.ts("dve", Sst[j][0].v, Sst[j][0].v, valid.v, ALU.mult)
            P.ts("dve", car_l.v, car_l.v, valid.v, ALU.mult)
            P.ts("dve", car_3.v, car_3.v, valid.v, ALU.mult)
        hT = A("hT", [128, 8, TG], BF16)
        if own:
            qT = A("qT", [128, 4, TG], BF16)
            yrw = A("yrw", [128, 4, TG], BF16)
            oT = A("oT", [128, 4, TG], BF16)
        WM = P.arena_off
        posi = A("posi", [128, 2], I32)
        P.dma("sp", posi.v, posd[:, gi * 2:gi * 2 + 2], key="posi")
        posf_ = A("posf", [128, 2], F32)
        P.copy("dve", posf_.v, posi.v)
        ang = A("ang", [128, 2, 32], F32)
        P.tt("dve", ang.v, posf_.v.re("p (t o) -> p t o", o=1).bc([128, 2, 32]),
             invf.v.re("p (o d) -> p o d", o=1).bc([128, 2, 32]), ALU.mult)
        cst = A("cst", [128, 2, 32], F32)
        snt = A("snt", [128, 2, 32], F32)
        for (dst_, off) in ((snt, 0.0), (cst, 0.25)):
            tq = A("tq", [128, 2, 32], F32)
            P.ts("dve", tq.v, ang.v, 1.0 / (2.0 * math.pi), ALU.mult, off, ALU.add)
            tqi = A("tqi", [128, 2, 32], I32)
            P.copy("dve", tqi.v, tq.v)
            P.copy("dve", tq.v, tqi.v)
            rr_ = A("rr_", [128, 2, 32], F32)
            P.stt("dve", rr_.v, tq.v, -6.28125, ang.v, ALU.mult, ALU.add)
            P.stt("dve", rr_.v, tq.v, -0.0019353071795864769, rr_.v, ALU.mult, ALU.add)
            if off != 0.0:
                P.ts("dve", rr_.v, rr_.v, math.pi / 2.0, ALU.add)
            P.ts("dve", rr_.v, rr_.v, 3.14159, ALU.min, -3.14159, ALU.max)
            P.act(dst_.v, rr_.v, AF.Sin)
        junk = A("junk", [128, D], BF16)
        for t in range(2):
            xt = A("xt%d" % t, [128, D], F32)
            P.dma("sp", xt.v, V(xsrc[src], xr3[src][lt0 + t]))
            rstd = rstd_of(xt.v, junk, "n1")
            xn = A("xn", [128, D], BF16)
            P.act(xn.v, xt.v, AF.Identity, scale=rstd.v)
            for hh in range(2):
                bk = nb()
                bkb = bk.v.cast(BF16)
                for k4 in range(4):
                    kc = hh * 4 + k4
                    P.tr(bkb[:, k4 * 128:(k4 + 1) * 128], xn[:, kc * 128:(kc + 1) * 128], identb.v, partial=(k4 > 0))
                for k4 in range(4):
                    kc = hh * 4 + k4
                    P.act(hT[:, kc, t * 128:(t + 1) * 128], bkb[:, k4 * 128:(k4 + 1) * 128], AF.Identity,
                          bias=sh1c[:, kc:kc + 1], scale=G1c[:, kc:kc + 1], partial=True)
        if dbg and gi == NGP:
            dump("hT", hT.v.re("p a b -> p (a b)"), [128, 8 * TG], BF16)
        wk = slab(wsl(w_in, 512, 512), 512)
        wv = slab(wsl(w_in, 1024, 512), 512)
        for t in range(2):
            T = gi * 2 + t
            tc_ = slice(t * 128, (t + 1) * 128)
            bk = nb()
            for kc in range(8):
                P.mm(bk.v, hT[:, kc, tc_], wk[:, kc, :], start=(kc == 0), stop=(kc == 7))
            qk_norm_rope(bk, kgb, cst[:, t, :], snt[:, t, :], kT, slice(T * 128, (T + 1) * 128))
            bk = nb()
            for kc in range(8):
                P.mm(bk.v, hT[:, kc, tc_], wv[:, kc, :], start=(kc == 0), stop=(kc == 7))
            P.act(Vp[:, T, :, 0:128], bk.v.re("p (h d) -> p h d", h=4), AF.Identity,
                  scale=(1.0 if own else valid.v), partial=True)
            if not own:
                P.ts("dve", Vp[:, T, :, 128:129], Vp[:, T, :, 128:129], valid.v, ALU.mult, partial=True)
        if own:
            wq = slab(wsl(w_in, 0, 512), 512)
            for t in range(2):
                tc_ = slice(t * 128, (t + 1) * 128)
                bk = nb()
                for kc in range(8):
                    P.mm(bk.v, hT[:, kc, tc_], wq[:, kc, :], start=(kc == 0), stop=(kc == 7))
                qk_norm_rope(bk, qgb, cst[:, t, :], snt[:, t, :], qT, tc_)
        if dbg and gi == NGP:
            dump("qT", qT.v.re("p a b -> p (a b)"), [128, 4 * TG], BF16)
            dump("kT", kT.v.re("p a b -> p (a b)"), [128, 4 * 2 * NTOK], BF16)
            dump("Vp", Vp.v.re("p a b c -> p (a b c)"), [128, 32 * 4 * 129], BF16)
        if stage <= 1:
            if gi == NGP:
                return finish(P, nc, outd, dumps)
            reset()
            continue
        reset(WM)

        def shift_evac(bk, dst, mucol, omucol, carry):
            P.act(dst, bk[:, 0:TG], AF.Identity, scale=omucol)
            P.stt("dve", dst[:, 1:TG], bk[:, 0:TG - 1], mucol, dst[:, 1:TG], ALU.mult, ALU.add, partial=True)
            P.stt("dve", dst[:, 0:1], carry, mucol, dst[:, 0:1], ALU.mult, ALU.add, partial=True)
            P.copy("dve", carry, bk[:, TG - 1:TG])

        wl = slab(wsl(w_in, 3072, 256), 256)
        xsl = A("xsl", [128, 2, TG], F32)
        for oc in range(2):
            bk = nb()
            for kc in range(8):
                P.mm(bk[:, 0:TG], wl[:, kc, oc * 128:(oc + 1) * 128], hT[:, kc, :], start=(kc == 0), stop=(kc == 7))
            shift_evac(bk, xsl[:, oc, :], mu[:, 12 + oc:13 + oc], omu[:, 12 + oc:13 + oc], car_l[:, oc:oc + 1])
        tw = A("tw", [64, TG], F32)
        P.act(tw.v, xsl[0:64, 0, :], AF.Tanh)
        sgx = A("sgx", [128, TG], F32)
        P.act(sgx.v, xsl[:, 1, :], AF.Sigmoid)
        for j in range(4):
            w3 = nslot()
            for i3 in range(3):
                P.dma("pool", w3[:, :, i3 * 128:(i3 + 1) * 128], wsl(w_in, 1536 + i3 * 512 + j * 128, 128),
                      key=w3.name, partial=(i3 > 0))
            xs3 = A("xs3", [128, 3, TG], F32)
            for i3 in range(3):
                bk = nb()
                for kc in range(8):
                    P.mm(bk[:, 0:TG], w3[:, kc, i3 * 128:(i3 + 1) * 128], hT[:, kc, :], start=(kc == 0),
                         stop=(kc == 7))
                cidx = i3 * 4 + j
                shift_evac(bk, xs3[:, i3, :], mu[:, cidx:cidx + 1], omu[:, cidx:cidx + 1], car_3[:, j, i3:i3 + 1])
            xr_, xk_, xv_ = xs3[:, 0, :], xs3[:, 1, :], xs3[:, 2, :]
            jc = slice(j * 128, (j + 1) * 128)
            bk = nb()
            P.mm(bk[:, 0:TG], wda[0:64, jc], tw.v)
            ld = A("ld", [128, TG], F32)
            P.act(ld.v, bk[:, 0:TG], AF.Sigmoid, bias=w0c[:, j:j + 1])
            P.ts("dve", ld.v, ld.v, -math.exp(-0.5), ALU.mult)
            bk = nb()
            P.mm(bk[:, 0:TG], wda[64:128, jc], xsl[64:128, 0, :])
            aa = A("aa", [128, TG], F32)
            P.act(aa.v, bk[:, 0:TG], AF.Sigmoid, bias=a0c[:, j:j + 1])
            if own:
                bk = nb()
                P.mm(bk[:, 0:TG], g2s[:, jc], sgx.v)
                gj = A("gj", [128, TG], F32)
                P.copy("act", gj.v, bk[:, 0:TG])
            kk = A("kk", [128, TG], F32)
            P.ts("dve", kk.v, xk_, kkc[:, j:j + 1], ALU.mult)
            sq = A("sq", [128, TG], F32)
            P.tt("dve", sq.v, kk.v, kk.v, ALU.mult)
            bk = nb()
            P.mm(bk[:, 0:TG], BLK1.v, sq.v)
            nrm = A("nrm", [128, TG], F32)
            P.act(nrm.v, bk[:, 0:TG], AF.Sqrt)
            P.ts("dve", nrm.v, nrm.v, 1e-12, ALU.max)
            P.recip(nrm.v, nrm.v)
            P.tt("dve", kk.v, kk.v, nrm.v, ALU.mult)
            km = A("km", [128, TG], F32)
            P.ts("dve", km.v, aa.v, kac[:, j:j + 1], ALU.mult, omka[:, j:j + 1], ALU.add)
            P.tt("dve", km.v, km.v, xk_, ALU.mult)
            bbv = A("bbv", [128, TG], F32)
            P.tt("dve", bbv.v, kk.v, aa.v, ALU.mult)
            ca = A("ca", [128, 4, 64], F32)
            cb = A("cb", [128, 4, 64], F32)
            c3 = lambda v_: v_.re("p (c s) -> p c s", c=4)
            cur = c3(ld.v)
            bufs = [ca, cb]
            for si, sh in enumerate((1, 2, 4, 8, 16, 32)):
                nx = bufs[si % 2]
                P.tt("dve", nx[:, :, sh:64], cur[:, :, sh:64], cur[:, :, 0:64 - sh], ALU.add)
                P.copy("dve", nx[:, :, 0:sh], cur[:, :, 0:sh], partial=True)
                cur = nx.v
            cum = cur
            Dinc = A("Dinc", [128, 4, 64], F32)
            Dinv = A("Dinv", [128, 4, 64], F32)
            Dexc = A("Dexc", [128, 4, 64], F32)
            P.act(Dinc.v, cum, AF.Exp)
            P.act(Dinv.v, cum, AF.Exp, scale=-1.0)
            P.tt("dve", Dexc.v, cum, c3(ld.v), ALU.subtract)
            P.act(Dexc.v, Dexc.v, AF.Exp)
            for hf in range(2):
                rws = slice(hf * 64, hf * 64 + 64)
                cl_ = slice(hf * 64, hf * 64 + 64)
                P.stt("dve", ARD[rws, :, cl_], c3(kk.v)[rws], -1.0, Dexc[rws], ALU.mult, ALU.mult, partial=True)
                if own:
                    P.tt("dve", ARD[rws, :, 128 + hf * 64:128 + hf * 64 + 64], c3(xr_)[rws], Dinc[rws], ALU.mult,
                         partial=True)
                P.tt("dve", BtD[rws, :, cl_], c3(bbv.v)[rws], Dinv[rws], ALU.mult, partial=True)
                P.tt("dve", KtD[rws, :, cl_], c3(km.v)[rws], Dinv[rws], ALU.mult, partial=True)
                P.copy("dve", VtD[rws, :, cl_], c3(xv_)[rws], partial=True)
            if own:
                rkr = A("rkr", [128, TG], F32)
                P.stt("dve", rkr.v, xr_, rkc[:, j:j + 1], km.v, ALU.mult, ALU.mult)
                yfm = A("yfm", [128, TG], F32)
            for c in range(4):
                S0 = Sst[j][(gi * 4 + c) % 2]
                S1 = Sst[j][(gi * 4 + c + 1) % 2]
                par = c % 2
                m128 = lambda nm: A("%s_%d" % (nm, par), [128, 128], F32 if nm in ("GT", "P3t") else BF16)
                ps1 = n256()
                P.mm(ps1.v, BtD[:, c, :], ARD[:, c, :])
                X = m128("X0")
                P.tt("dve", X.v, ps1[:, 0:128], MUs.v, ALU.mult)
                ps2 = n256()
                P.mm(ps2.v, KtD[:, c, :], ARD[:, c, :])
                AKt = m128("AKt")
                P.tt("dve", AKt.v, ps2[:, 0:128], MUs.v, ALU.mult)
                if own:
                    RBt = m128("RBt")
                    P.tt("dve", RBt.v, ps1[:, 128:256], MUi.v, ALU.mult)
                    RKt = m128("RKt")
                    P.tt("dve", RKt.v, ps2[:, 128:256], MUi.v, ALU.mult)
                ps3 = n128()
                P.mm(ps3.v, ARD[:, c, 0:128], BtD[:, c, :])
                Y = m128("Y0")
                P.tt("dve", Y.v, ps3.v, MLs.v, ALU.mult)
                Tt = m128("Tt0")
                Tl = m128("Tl0")
                P.tt("dve", Tt.v, X.v, ident.v, ALU.add)
                P.tt("dve", Tl.v, Y.v, ident.v, ALU.add)
                for lvl in range(1, 6):
                    px = n128()
                    P.mm(px.v, Y.v, X.v)
                    Xn = m128("X%d" % lvl)
                    P.copy("act", Xn.v, px.v)
                    if lvl < 5:
                        py = n128()
                        P.mm(py.v, X.v, Y.v)
                        Yn = m128("Y%d" % lvl)
                        P.copy("act", Yn.v, py.v)
                    pt = n128()
                    P.mm(pt.v, Tl.v, Xn.v)
                    Ttn = m128("Tt%d" % lvl)
                    P.tt("dve", Ttn.v, pt.v, Tt.v, ALU.add)
                    if lvl < 5:
                        pl = n128()
                        P.mm(pl.v, Xn.v, Tl.v)
                        Tln = m128("Tl%d" % lvl)
                        P.tt("dve", Tln.v, pl.v, Tl.v, ALU.add)
                        Tl = Tln
                        Y = Yn
                    Tt = Ttn
                    X = Xn
                tms = []
                for nm, srcv in (("Atm", ARD[:, c, 0:128]), ("Btm", BtD[:, c, :]), ("Ktm", KtD[:, c, :]),
                                 ("Vtm", VtD[:, c, :])):
                    pp = n128()
                    ppb = pp.v.cast(BF16)[:, 0:128]
                    P.tr(ppb, srcv, identb.v)
                    tt_ = m128(nm)
                    P.copy("act", tt_.v, ppb)
                    tms.append(tt_)
                Atm, Btm, Ktm, Vtm = tms
                pp = n128()
                P.mm(pp.v, Tt.v, Atm.v)
                P1 = m128("P1")
                P.copy("act", P1.v, pp.v)
                pp = n128()
                P.mm(pp.v, AKt.v, Vtm.v)
                W2 = m128("W2")
                P.copy("dve", W2.v, pp.v)
                pp = n128()
                P.mm(pp.v, Tt.v, W2.v)
                P2 = m128("P2")
                P.copy("act", P2.v, pp.v)
                pp = n128()
                P.mm(pp.v, P1.v, Btm.v)
                GT = m128("GT")
                P.tt("dve", GT.v, pp.v, ident.v, ALU.add)
                if own:
                    pp = n128()
                    P.mm(pp.v, P1.v, RBt.v)
                    P3t = m128("P3t")
                    P.tt("dve", P3t.v, pp.v, ARD[:, c, 128:256], ALU.add)
                    py_ = n128()
                    P.mm(py_.v, P2.v, RBt.v, start=True, stop=False)
                    P.mm(py_.v, Vtm.v, RKt.v, start=False, stop=False)
                    P.mm(py_.v, S0.v, P3t.v, start=False, stop=True)
                    P.copy("act", yfm[0:64, c * 64:(c + 1) * 64], py_[0:64, 0:64], partial=True)
                    P.copy("act", yfm[64:128, c * 64:(c + 1) * 64], py_[64:128, 64:128], partial=True)
                pss = n128()
                P.mm(pss.v, Btm.v, P2.v, start=True, stop=False)
                P.mm(pss.v, Ktm.v, Vtm.v, start=False, stop=False)
                P.mm(pss.v, GT.v, S0.v, start=False, stop=True)
                P.act(S1.v, pss.v, AF.Identity, scale=Dinc[:, c, 63:64])
            if own:
                bk = nb()
                P.mm(bk[:, 0:TG], BLKm.v, yfm.v)
                cen = A("cen", [128, TG], F32)
                P.tt("dve", cen.v, yfm.v, bk[:, 0:TG], ALU.subtract)
                sq2 = A("sq2", [128, TG], F32)
                P.tt("dve", sq2.v, cen.v, cen.v, ALU.mult)
                bk = nb()
                P.mm(bk[:, 0:TG], BLKm.v, sq2.v)
                rsg = A("rstdg", [128, TG], F32)
                P.act(rsg.v, bk[:, 0:TG], AF.Sqrt, bias=epsg.v)
                P.recip(rsg.v, rsg.v)
                P.tt("dve", cen.v, cen.v, rsg.v, ALU.mult)
                P.ts("dve", cen.v, cen.v, lnwc[:, j:j + 1], ALU.mult, lnbc[:, j:j + 1], ALU.add)
                bk = nb()
                P.mm(bk[:, 0:TG], BLK1.v, rkr.v)
                bon = A("bon", [128, TG], F32)
                P.tt("dve", bon.v, bk[:, 0:TG], xv_, ALU.mult)
                P.tt("dve", cen.v, cen.v, bon.v, ALU.add)
                P.tt("dve", yrw[:, j, :], cen.v, gj.v, ALU.mult, partial=(j > 0))
        if dbg and gi == NGP:
            dump("yrw", yrw.v.re("p a b -> p (a b)"), [128, 4 * TG], BF16)
        if stage <= 2:
            if gi == NGP:
                return finish(P, nc, outd, dumps)
            reset()
            continue
        if not own:
            reset()
            continue
        reset(WM)

        oatt = A("oatt", [128, 2, 512], BF16)
        osb = A("osb", [128, 4, 129], F32)
        nkb = 16 + og * 2 + 2
        for h in range(4):
            for s in range(2):
                accs = [r256[s * 2 + i] for i in range(2)]
                prs = slice(s * 64, s * 64 + 64)
                for kb in range(nkb):
                    bk = nb()
                    P.mm(bk[:, 0:TG], kT[prs, h, kb * 128:(kb + 1) * 128], qT[prs, h, :])
                    E = A("E%d" % (kb % 2), [128, TG], BF16)
                    P.act(E.v, bk[:, 0:TG], AF.Exp, scale=0.125)
                    for i in range(2):
                        oi = og * 2 + i
                        lastkb = 16 + oi
                        if kb > lastkb:
                            continue
                        if kb == lastkb:
                            P.memset("dve", E[64:128, i * 128:i * 128 + 64], 0.0)
                        P.mm(accs[i][:, 0:129], E[:, i * 128:(i + 1) * 128], Vp[:, kb, h, :],
                             start=(kb == 0), stop=(kb == lastkb))
                for i in range(2):
                    P.copy("act", osb[:, s * 2 + i, :], accs[i][:, 0:129], partial=True)
            for i in range(2):
                r0 = A("r0", [128, 1], F32)
                r1 = A("r1", [128, 1], F32)
                P.recip(r0.v, osb[:, i, 128:129])
                P.recip(r1.v, osb[:, 2 + i, 128:129])
                P.tt("dve", r1.v, r1.v, lamc.v, ALU.mult)
                t0 = A("t0", [128, 128], F32)
                P.ts("dve", t0.v, osb[:, i, 0:128], r0.v, ALU.mult)
                P.stt("dve", t0.v, osb[:, 2 + i, 0:128], r1.v, t0.v, ALU.mult, ALU.add)
                ssq = A("assq", [128, 1], F32)
                P.memset("dve", ssq.v, 0.0)
                junk2 = A("junk2", [128, 128], F32)
                P.act(junk2.v, t0.v, AF.Square, accum=ssq.v)
                P.act(ssq.v, ssq.v, AF.Sqrt, bias=epss.v, scale=1.0 / 128.0)
                P.recip(ssq.v, ssq.v)
                P.stt("dve", oatt[:, i, h * 128:(h + 1) * 128], t0.v, ssq.v, sgb.v, ALU.mult, ALU.mult,
                      partial=True)
        for i in range(2):
            bk = nb()
            bkb = bk.v.cast(BF16)
            for hc in range(4):
                P.tr(bkb[:, hc * 128:(hc + 1) * 128], oatt[:, i, hc * 128:(hc + 1) * 128], identb.v, partial=(hc > 0))
            P.copy("act", oT[:, :, i * 128:(i + 1) * 128], bkb[:, 0:512].re("p (a t) -> p a t", a=4), partial=True)
        if dbg and gi == NGP:
            dump("oT", oT.v.re("p a b -> p (a b)"), [128, 4 * TG], BF16)
        if stage <= 3:
            if gi == NGP:
                return finish(P, nc, outd, dumps)
            reset()
            continue
        reset(WM)

        gtb = A("gtb", [128, 2, D], F32)
        P.dma("sp", gtb[:, 0, :], pb(gts, (slice(0, 1), slice(None))), key="gtb")
        P.dma("sp", gtb[:, 1, :], pb(gts, (slice(1, 2), slice(None))), key="gtb", partial=True)
        x1 = A("x1", [128, 2, D], F32)
        WM3 = P.arena_off
        bgb16 = A("bgb16", [1, 2048], BF16)
        P.dma("pool", bgb16.v, bgate_row.v, key="bgb16")
        gates = A("gates", [128, 2, 2048], BF16)
        for cs in range(4):
            wg = slab(wsl(w_gate, cs * 512, 512), 512)
            for i in range(2):
                bk = nb()
                for kc in range(8):
                    P.mm(bk.v, hT[:, kc, i * 128:(i + 1) * 128], wg[:, kc, :], start=(kc == 0), stop=False)
                P.mm(bk.v, onesrow.v, bgb16[0:1, cs * 512:(cs + 1) * 512], start=False, stop=True)
                P.act(gates[:, i, cs * 512:(cs + 1) * 512], bk.v, AF.Sigmoid, partial=True)
        mm_ = A("mm_", [128, 2, D], F32)
        wba = slab4(w_ba)
        for i in range(2):
            for hf in range(2):
                bk = nb()
                for hc in range(4):
                    P.mm(bk.v, oT[:, hc, i * 128:(i + 1) * 128], wba[:, hc, hf * 512:(hf + 1) * 512],
                         start=(hc == 0), stop=(hc == 3))
                P.tt("dve", mm_[:, i, hf * 512:(hf + 1) * 512], bk.v, gates[:, i, hf * 512:(hf + 1) * 512],
                     ALU.mult, partial=True)
        wbb = slab4(w_bb)
        mb = A("mb", [128, 2, D], BF16)
        for i in range(2):
            for hf in range(2):
                bk = nb()
                for hc in range(4):
                    P.mm(bk.v, yrw[:, hc, i * 128:(i + 1) * 128], wbb[:, hc, hf * 512:(hf + 1) * 512],
                         start=(hc == 0), stop=(hc == 3))
                tmpg = A("tmpg", [128, 512], F32)
                P.tt("dve", tmpg.v, bk.v, gates[:, i, 1024 + hf * 512:1024 + (hf + 1) * 512], ALU.mult)
                P.tt("dve", mb[:, i, hf * 512:(hf + 1) * 512], tmpg.v, mm_[:, i, hf * 512:(hf + 1) * 512],
                     ALU.add, partial=True)
        mT = A("mT", [128, 8, TG], BF16)
        for i in range(2):
            for hh in range(2):
                bk = nb()
                bkb = bk.v.cast(BF16)
                for k4 in range(4):
                    kc = hh * 4 + k4
                    P.tr(bkb[:, k4 * 128:(k4 + 1) * 128], mb[:, i, kc * 128:(kc + 1) * 128], identb.v, partial=(k4 > 0))
                P.copy("act", mT[:, hh * 4:(hh + 1) * 4, i * 128:(i + 1) * 128],
                       bkb[:, 0:512].re("p (a t) -> p a t", a=4), partial=True)
        for i in range(2):
            P.dma("sp", x1[:, i, :], V(xo, xr3["o"][lt0 + i]), key="x1", partial=(i > 0))
        for cs in range(2):
            wo = slab(wsl(w_out, cs * 512, 512), 512)
            for i in range(2):
                bk = nb()
                for kc in range(8):
                    P.mm(bk.v, mT[:, kc, i * 128:(i + 1) * 128], wo[:, kc, :], start=(kc == 0), stop=(kc == 7))
                tmpg = A("tmpg", [128, 512], F32)
                P.tt("dve", tmpg.v, bk.v, gtb[:, 0, cs * 512:(cs + 1) * 512], ALU.mult)
                P.tt("dve", x1[:, i, cs * 512:(cs + 1) * 512], tmpg.v, x1[:, i, cs * 512:(cs + 1) * 512], ALU.add,
                     partial=True)
        if dbg and gi == NGP:
            dump("x1", x1.v.re("p a b -> p (a b)"), [128, 2 * D])
        reset(WM3)
        h2T32 = A("h2T32", [128, 8, TG], F32)
        h2Tb = A("h2Tb", [128, 8, TG], BF16)
        junk = A("junkb", [128, D], BF16)
        for i in range(2):
            rstd = rstd_of(x1[:, i, :], junk, "n2")
            xn2 = A("xn2", [128, D], F32)
            P.act(xn2.v, x1[:, i, :], AF.Identity, scale=rstd.v)
            xnb = A("xnb%d" % i, [128, D], BF16)
            P.copy("dve", xnb.v, xn2.v)
            row0 = (lt0 + i) * 128
            P.dma("sp", h2s[row0:row0 + 128, :], xnb.v, partial=True)
            for hh in range(2):
                bk = nb()
                for k4 in range(4):
                    kc = hh * 4 + k4
                    P.tr(bk[:, k4 * 128:(k4 + 1) * 128], xn2[:, kc * 128:(kc + 1) * 128], ident.v, partial=(k4 > 0))
                for k4 in range(4):
                    kc = hh * 4 + k4
                    P.act(h2T32[:, kc, i * 128:(i + 1) * 128], bk[:, k4 * 128:(k4 + 1) * 128], AF.Identity,
                          bias=sh2c[:, kc:kc + 1], scale=G2c[:, kc:kc + 1], partial=True)
        P.copy("dve", h2Tb.v, h2T32.v)
        wrs = nslot()
        wr32 = wrs.v.re("p a b -> p (a b)").cast(F32).re("p (a b) -> p a b", a=8)
        P.dma("pool", wr32, V(w_router, w_router.full.rearrange("(kc p) n -> p kc n", p=128)), key=wrs.name)
        for i in range(2):
            oi = og * 2 + i
            bk = nb()
            for kc in range(8):
                P.mm(bk[:, 0:NE], h2T32[:, kc, i * 128:(i + 1) * 128], V(wrs, wr32.ap[:, kc, :]), start=(kc == 0),
                     stop=(kc == 7))
            sc = A("sc", [128, NE], F32)
            P.act(sc.v, bk[:, 0:NE], AF.Sigmoid)
            bia = A("bia", [128, 8, 32], F32)
            P.tt("dve", bia.v, sc.v.re("p (g e) -> p g e", g=8), rbb.v.re("p (g e) -> p g e", g=8), ALU.add)
            m8g = A("m8g", [128, 8, 8], F32)
            for g in range(8):
                P.max8(m8g[:, g, :], bia[:, g, :], partial=(g > 0))
            gs = A("gs", [128, 8], F32)
            P.tt("dve", gs.v.re("p (g o) -> p g o", o=1), m8g[:, :, 0:1], m8g[:, :, 1:2], ALU.add)
            gm8 = A("gm8", [128, 8], F32)
            P.max8(gm8.v, gs.v)
            gmask = A("gmask", [128, 8], F32)
            P.ts("dve", gmask.v, gs.v, gm8[:, 3:4], ALU.is_ge)
            neg = A("neg", [128, 8], F32)
            P.ts("dve", neg.v, gmask.v, 1e9, ALU.mult, -1e9, ALU.add)
            msk = A("msk", [128, 8, 32], F32)
            P.tt("dve", msk.v, bia.v, gmask.v.re("p (g o) -> p g o", o=1).bc([128, 8, 32]), ALU.mult)
            P.tt("dve", msk.v, msk.v, neg.v.re("p (g o) -> p g o", o=1).bc([128, 8, 32]), ALU.add)
            mskf = msk.v.re("p g e -> p (g e)")
            e8 = A("e8", [128, 8], F32)
            P.max8(e8.v, mskf)
            sel = A("sel", [128, NE], F32)
            P.ts("dve", sel.v, mskf, e8[:, 7:8], ALU.is_ge)
            P.copy("dve", selall[:, oi, :], sel.v, partial=True)
            wsel = A("wsel", [128, NE], F32)
            P.tt("dve", wsel.v, sel.v, sc.v, ALU.mult)
            den = A("den", [128, 1], F32)
            P.reduce(den.v, wsel.v)
            P.recip(den.v, den.v)
            wdn = A("wdn%d" % i, [128, NE], F32)
            P.ts("dve", wdn.v, wsel.v, den.v, ALU.mult, 2.5, ALU.mult)
            row0 = (lt0 + i) * 128
            P.dma("sp", wscr[row0:row0 + 128, :], wdn.v, partial=True)
        wsu = slab(wsl(w_sug, 0, 512), 512)
        hidT = A("hidT", [128, 2, TG], BF16)
        for i in range(2):
            bk = nb()
            for kc in range(8):
                P.mm(bk.v, h2Tb[:, kc, i * 128:(i + 1) * 128], wsu[:, kc, :], start=(kc == 0), stop=(kc == 7))
            sg = A("sg", [128, 256], F32)
            P.act(sg.v, bk[:, 0:256], AF.Silu)
            hid = A("hid", [128, 256], BF16)
            P.tt("dve", hid.v, sg.v, bk[:, 256:512], ALU.mult)
            bk2 = nb()
            bkb = bk2.v.cast(BF16)
            for fc in range(2):
                P.tr(bkb[:, fc * 128:(fc + 1) * 128], hid[:, fc * 128:(fc + 1) * 128], identb.v, partial=(fc > 0))
            P.copy("act", hidT[:, :, i * 128:(i + 1) * 128], bkb[:, 0:256].re("p (a t) -> p a t", a=2), partial=True)
        wsds = nslot()
        wsd = wsds.v.re("p a b -> p (a b)").re("p (a b) -> p a b", a=4)[:, 0:2, :]
        P.dma("pool", wsd, V(w_sd, w_sd.full.rearrange("(fc p) n -> p fc n", p=128)), key=wsds.name)
        for i in range(2):
            bs = A("bs%d" % i, [128, D], F32)
            for hf in range(2):
                bk = nb()
                for fc in range(2):
                    P.mm(bk.v, hidT[:, fc, i * 128:(i + 1) * 128], wsd[:, fc, hf * 512:(hf + 1) * 512],
                         start=(fc == 0), stop=(fc == 1))
                tmpg = A("tmpg", [128, 512], F32)
                P.tt("dve", tmpg.v, bk.v, gtb[:, 1, hf * 512:(hf + 1) * 512], ALU.mult)
                P.tt("dve", bs[:, hf * 512:(hf + 1) * 512], tmpg.v, x1[:, i, hf * 512:(hf + 1) * 512], ALU.add,
                     partial=(hf > 0))
            row0 = (lt0 + i) * 128
            P.dma("sp", bases[row0:row0 + 128, :], bs.v, partial=True)
        if dbg and gi == NGP:
            dump("h2T32", h2T32.v.re("p a b -> p (a b)"), [128, 8 * TG])
            dump("sel0", selall[:, 0, :], [128, NE], BF16)
        if stage <= 4 and gi == NGP:
            return finish(P, nc, outd, dumps)
        reset()

    if stage <= 4:
        return finish(P, nc, outd, dumps)

    gt2b = A("gt2b", [128, D], F32)
    P.dma("sp", gt2b.v, pb(gts, (slice(1, 2), slice(None))), key="gt2b")
    idxT = A("idxT", [128, NBLK, NE], I32)
    idx8 = A("idx8", [128, 16, 8], I32)
    WM2 = P.arena_off
    revb = A("revb", [128, NTOK], F32)
    P.add("pool", lambda e: e.iota(revb.full, [[-1, NTOK]], base=4096, channel_multiplier=0,
                                   allow_small_or_imprecise_dtypes=True), [], [revb.v])
    eoff = A("eoff", [128, NE], F32)
    P.add("pool", lambda e: e.iota(eoff.full, [[CAP, NE]], base=1, channel_multiplier=0,
                                   allow_small_or_imprecise_dtypes=True), [], [eoff.v])
    keyT = A("keyT", [128, 2, NTOK], F32)
    for eh in range(2):
        for q4 in range(2):
            bk = nb()
            bkb = bk.v.cast(BF16)
            for t8 in range(8):
                ti = q4 * 8 + t8
                P.tr(bkb[:, t8 * 128:(t8 + 1) * 128], selall[:, ti, eh * 128:(eh + 1) * 128], identb.v,
                     partial=(t8 > 0))
            P.tt("dve", keyT[:, eh, q4 * 1024:(q4 + 1) * 1024], bkb[:, 0:1024], revb[:, q4 * 1024:(q4 + 1) * 1024],
                 ALU.mult, partial=True)
    top = A("top", [128, 2, CAP], F32)
    for eh in range(2):
        for r in range(CAP // 8):
            P.max8(top[:, eh, r * 8:(r + 1) * 8], keyT[:, eh, :], partial=True)
            P.add("dve", (lambda eh_, r_: (lambda e: e.match_replace(
                out=keyT.full[:, eh_, :], in_to_replace=top.full[:, eh_, r_ * 8:(r_ + 1) * 8],
                in_values=keyT.full[:, eh_, :], imm_value=0.0)))(eh, r), [top.v, keyT.v], [keyT.v])
    P.ts("dve", top.v, top.v, -1.0, ALU.mult, 4096.0, ALU.add)
    P.ts("dve", top.v, top.v, float(NTOK), ALU.min)
    for eh in range(2):
        for blk in range(NBLK):
            bk = nb()
            P.tr(bk[:, 0:128], top[:, eh, blk * 128:(blk + 1) * 128], ident.v)
            P.copy("dve", idxT[:, blk, eh * 128:(eh + 1) * 128], bk[:, 0:128], partial=(eh + blk > 0))
    for ti in range(16):
        bk = nb()
        P.mm(bk[:, 0:NE], TRISb.v, selall[:, ti, :], start=True, stop=(ti == 0))
        for tp in range(ti):
            P.mm(bk[:, 0:NE], ONESb.v, selall[:, tp, :], start=False, stop=(tp == ti - 1))
        okm = A("okm", [128, NE], F32)
        P.ts("dve", okm.v, bk[:, 0:NE], float(CAP), ALU.is_lt)
        fl = A("fl", [128, NE], F32)
        P.tt("dve", fl.v, bk[:, 0:NE], eoff.v, ALU.add)
        P.tt("dve", fl.v, fl.v, okm.v, ALU.mult)
        P.tt("dve", fl.v, fl.v, selall[:, ti, :], ALU.mult)
        t8v = A("t8v", [128, 8], F32)
        P.max8(t8v.v, fl.v)
        isz = A("isz", [128, 8], F32)
        P.ts("dve", isz.v, t8v.v, 0.0, ALU.is_equal, float(DUMMY_Y + 1), ALU.mult)
        P.stt("dve", t8v.v, t8v.v, -1.0, isz.v, ALU.add, ALU.add)
        P.copy("dve", idx8[:, ti, :], t8v.v, partial=(ti > 0))
    if dbg:
        dump("idxT", idxT.v.re("p a b -> p (a b)"), [128, NBLK * NE], I32)
        dump("idx8", idx8.v.re("p a b -> p (a b)"), [128, 128], I32)
    if stage <= 5:
        return finish(P, nc, outd, dumps)

    wdring = [A("wdr%d" % i, [128, 2, D], BF16) for i in range(3)]
    for e in range(NE):
        wug = slab(V(w_eug, w_eug.full[e].rearrange("(kc p) n -> p kc n", p=128)), 512)
        wd = wdring[e % 3]
        P.dma("pool", wd.v, V(w_ed, w_ed.full[e].rearrange("(fc p) n -> p fc n", p=128)))
        for blk in range(NBLK):
            eb = e * NBLK + blk
            Xe = A("Xe%d" % (eb % 2), [128, D], BF16)
            P.gather(Xe.v, h2s.v, idxT[:, blk, e:e + 1])
            We = A("We%d" % (eb % 2), [128, NE], F32)
            P.gather(We.v, wscr.v, idxT[:, blk, e:e + 1])
            XeT = A("XeT%d" % (eb % 2), [128, 8, 128], BF16)
            for hh in range(2):
                bk = nb()
                bkb = bk.v.cast(BF16)
                for k4 in range(4):
                    kc = hh * 4 + k4
                    P.tr(bkb[:, k4 * 128:(k4 + 1) * 128], Xe[:, kc * 128:(kc + 1) * 128], identb.v, partial=(k4 > 0))
                for k4 in range(4):
                    kc = hh * 4 + k4
                    P.act(XeT[:, kc, :], bkb[:, k4 * 128:(k4 + 1) * 128], AF.Identity,
                          bias=sh2c[:, kc:kc + 1], scale=G2c[:, kc:kc + 1], partial=(kc > 0))
            bk = nb()
            for kc in range(8):
                P.mm(bk.v, XeT[:, kc, :], wug[:, kc, :], start=(kc == 0), stop=(kc == 7))
            sg = A("esg", [128, 256], F32)
            P.act(sg.v, bk[:, 0:256], AF.Silu)
            hid = A("ehid", [128, 256], BF16)
            P.tt("dve", hid.v, sg.v, bk[:, 256:512], ALU.mult)
            bk2 = nb()
            bkb = bk2.v.cast(BF16)
            for fc in range(2):
                P.tr(bkb[:, fc * 128:(fc + 1) * 128], hid[:, fc * 128:(fc + 1) * 128], identb.v, partial=(fc > 0))
            ehT = A("ehT", [128, 2, 128], BF16)
            P.copy("act", ehT.v, bkb[:, 0:256].re("p (a t) -> p a t", a=2))
            ysb = A("ysb%d" % (eb % 2), [128, D], BF16)
            for hf in range(2):
                bk = nb()
                for fc in range(2):
                    P.mm(bk.v, ehT[:, fc, :], wd[:, fc, hf * 512:(hf + 1) * 512], start=(fc == 0), stop=(fc == 1))
                if hf == 0:
                    P.act(ysb[:, 0:512], bk.v, AF.Identity, scale=We[:, e:e + 1])
                else:
                    P.ts("dve", ysb[:, 512:1024], bk.v, We[:, e:e + 1], ALU.mult, partial=True)
            P.dma("sp", yscr[e * CAP + blk * 128:e * CAP + (blk + 1) * 128, :], ysb.v, partial=True)
    reset(WM2)

    for ti in range(16):
        acc = A("acc%d" % (ti % 2), [128, D], F32)
        for j in range(8):
            G = A("G%d" % (j % 4), [128, D], BF16)
            P.gather(G.v, yscr.v, idx8[:, ti, j:j + 1])
            if j == 0:
                P.copy("act", acc.v, G.v)
            else:
                P.tt("dve", acc.v, acc.v, G.v, ALU.add)
        bt = A("bt%d" % (ti % 2), [128, D], F32)
        P.dma("sp", bt.v, bases[ti * 128:(ti + 1) * 128, :])
        P.tt("dve", acc.v, acc.v, gt2b.v, ALU.mult)
        P.tt("dve", bt.v, bt.v, acc.v, ALU.add)
        P.dma("sp", outd[ti * 128:(ti + 1) * 128, :], bt.v, partial=True)
    return finish(P, nc, outd, dumps)


def finish(P, nc, outd, dumps):
    outs = [t.v for t in dumps.values()]
    P.wait("sp", outs + [outd.v])
    P.emit()
    return nc, P, dumps


def col(v, n):
    return np.ascontiguousarray(np.asarray(v, np.float32).reshape(n, 128).T)


def make_in_maps(inp, cores=range(8)):
    f = lambda k: np.ascontiguousarray(np.asarray(inp[k])[0])
    shared = {
        "w_ada": f("w_ada"), "bada_row": f("b_ada").reshape(1, -1), "bada_col": col(f("b_ada"), 48),
        "n1g_col": col(f("norm1_g"), 8), "n2g_col": col(f("norm2_g"), 8),
        "w_in": f("w_in"), "w_gate": f("w_gate"), "bgate_row": f("b_gate").reshape(1, -1),
        "qg_row": f("q_norm_g").reshape(1, -1), "kg_row": f("k_norm_g").reshape(1, -1),
        "lam_rows": np.stack([f("lambda_q1"), f("lambda_k1"), f("lambda_q2"), f("lambda_k2")]).astype(np.float32),
        "subg_row": f("subln_g").reshape(1, -1),
        "mu_col": col(f("rwkv_mu"), 14), "w0_col": col(f("w_decay0"), 4), "wd2": f("w_decay2"),
        "a0_col": col(f("a0"), 4), "a2d": f("a2"), "g2d": f("g2"),
        "kk_col": col(f("k_k"), 4), "ka_col": col(f("k_a"), 4), "rk_col": col(f("r_k").reshape(-1), 4),
        "lnw_col": col(f("ln_x_w"), 4), "lnb_col": col(f("ln_x_b"), 4),
        "w_ba": f("w_branch_a"), "w_bb": f("w_branch_b"), "w_out": f("w_out"),
        "w_router": f("w_router"), "rbias_row": f("router_bias").reshape(1, -1),
        "w_eug": f("w_expert_up_gate"), "w_ed": f("w_expert_down"),
        "w_sug": f("w_shared_up_gate"), "w_sd": f("w_shared_down"),
        "invfd": (1.0 / (10000.0 ** (np.arange(0, 64, 2, dtype=np.float32) / 64.0))).astype(np.float32).reshape(1, 32),
    }
    x = np.asarray(inp["x"])
    c = np.asarray(inp["c"])
    pos = np.asarray(inp["positions"])
    maps = []
    for core in cores:
        b, half = core // 2, core % 2
        m = dict(shared)
        m["xo"] = np.ascontiguousarray(x[b, half * NTOK:(half + 1) * NTOK])
        m["xp"] = np.ascontiguousarray(x[b, 0:NTOK])
        pp = np.concatenate([pos[b, 0:NTOK], pos[b, half * NTOK:(half + 1) * NTOK]]).astype(np.int32)
        m["posd"] = np.ascontiguousarray(pp.reshape(32, 128).T)
        m["ccol"] = col(c[b], 8)
        m["validd"] = np.full((128, 1), float(half), np.float32)
        maps.append(m)
    return maps


_CACHE = {}


def kernel(**inputs):
    if "nc" not in _CACHE:
        _CACHE["nc"] = build()[0]
    nc = _CACHE["nc"]
    maps = make_in_maps(inputs)
    res = run_bass_kernel_spmd(nc, maps, core_ids=list(range(8)))
    out = np.zeros((4, 4096, D), np.float32)
    for core in range(8):
        b, half = core // 2, core % 2
        out[b, half * NTOK:(half + 1) * NTOK] = res.results[core]["out"]
    return out
```
